# Optimizing a Trainium2 kernel written in Bass

```python
import math
import jax, jax.numpy as jnp
from jax import lax
import numpy as np

D_MODEL = 1024
BATCH = 2
SEQ = 8192
DEPTH = 1

N_HEADS_A = 8
HEAD_DIM = 64
WIDTH_A = N_HEADS_A * HEAD_DIM
IDX_HEADS = 8
IDX_DIM = 64
TOPK_MAX = 256
N_HEADS_B = 8
WIDTH_B = N_HEADS_B * HEAD_DIM
N_BUCKETS = 32
MAX_DISTANCE = 128
PEER_HEADS = 8
N_KEYS = 128
N_EXPERTS = N_KEYS * N_KEYS
PEER_QDIM = 256
PEER_HALF = PEER_QDIM // 2
PEER_TOPK = 16
Q_BLOCK = 128
TOK_CHUNK = 128
EPS = 1e-6
IN_SPLIT_SIZES = (WIDTH_A, WIDTH_A, WIDTH_A,
                  IDX_HEADS * IDX_DIM, IDX_DIM, IDX_HEADS,
                  WIDTH_B, WIDTH_B, WIDTH_B,
                  D_MODEL, D_MODEL)
IN_WIDTH = 3 * WIDTH_A + IDX_HEADS * IDX_DIM + IDX_DIM + IDX_HEADS + 3 * WIDTH_B + 2 * D_MODEL

kernel_name = "hybrid_dsa_stickbreak_peer_block"


def rmsnorm(x, g):
    xf = x.astype(jnp.float32)
    y = xf * lax.rsqrt(jnp.mean(xf * xf, axis=-1, keepdims=True) + EPS)
    return (y * g.astype(jnp.float32)).astype(x.dtype)


def modulate(h, shift, scale):
    return h * (1 + scale[:, None, :]) + shift[:, None, :]


def t5_bucket(n):
    max_exact = N_BUCKETS // 2
    nf = jnp.maximum(n, 1).astype(jnp.float32)
    large = max_exact + (jnp.log(nf / max_exact) / math.log(MAX_DISTANCE / max_exact)
                         * (N_BUCKETS - max_exact)).astype(jnp.int32)
    large = jnp.minimum(large, N_BUCKETS - 1)
    return jnp.where(n < max_exact, n, large)


def to_blocks(a, nblk):
    b = a.shape[0]
    return a.reshape(b, nblk, Q_BLOCK, *a.shape[2:]).swapaxes(0, 1)


def dsa_attention(q, k, v, qi, ki, wi, rel_bias):
    b, s, h, dh = q.shape
    topk = min(TOPK_MAX, s // 4)
    nblk = s // Q_BLOCK
    key_pos = jnp.arange(s)
    idx_scale = (IDX_DIM ** -0.5) * (IDX_HEADS ** -0.5)

    def one_block(args):
        blk, qb, qib, wib = args
        q_pos = blk * Q_BLOCK + jnp.arange(Q_BLOCK)
        rel = jax.nn.relu(jnp.einsum('bthd,bsd->bths', qib, ki))
        score = jnp.einsum('bths,bth->bts', rel, wib).astype(jnp.float32) * idx_scale
        causal = key_pos[None, :] <= q_pos[:, None]
        score = jnp.where(causal[None], score, -jnp.inf)
        top_score, idx = lax.top_k(score, topk)
        valid = jnp.isfinite(top_score)
        kg = jax.vmap(lambda kk, ii: kk[ii])(k, idx)
        vg = jax.vmap(lambda vv, ii: vv[ii])(v, idx)
        logits = jnp.einsum('bthd,btkhd->bhtk', qb, kg).astype(jnp.float32) * (dh ** -0.5)
        bias = rel_bias[t5_bucket(q_pos[None, :, None] - idx)].astype(jnp.float32)
        logits = logits + jnp.moveaxis(bias, -1, 1)
        logits = jnp.where(valid[:, None], logits, -jnp.inf)
        p = jax.nn.softmax(logits, axis=-1).astype(v.dtype)
        return jnp.einsum('bhtk,btkhd->bthd', p, vg)

    out = lax.map(one_block, (jnp.arange(nblk), to_blocks(q, nblk), to_blocks(qi, nblk), to_blocks(wi, nblk)))
    return out.swapaxes(0, 1).reshape(b, s, h * dh)


def stick_breaking_attention(q, k, v):
    b, s, h, dh = q.shape
    nblk = s // Q_BLOCK
    key_pos = jnp.arange(s)

    def one_block(args):
        blk, qb = args
        q_pos = blk * Q_BLOCK + jnp.arange(Q_BLOCK)
        z = jnp.einsum('bthd,bshd->bhts', qb, k).astype(jnp.float32) * (dh ** -0.5)
        strict = (key_pos[None, :] < q_pos[:, None])[None, None]
        log_beta = jax.nn.log_sigmoid(z)
        log_one_minus = jnp.where(strict, log_beta - z, 0.0)
        suffix = lax.cumsum(log_one_minus, axis=3, reverse=True) - log_one_minus
        a = jnp.where(strict, jnp.exp(log_beta + suffix), 0.0).astype(v.dtype)
        return jnp.einsum('bhts,bshd->bthd', a, v)

    out = lax.map(one_block, (jnp.arange(nblk), to_blocks(q, nblk)))
    return out.swapaxes(0, 1).reshape(b, s, h * dh)


def peer_ffn(h, w_q, sub_keys, u, v):
    t, d = h.shape
    qh = (h @ w_q).reshape(t, PEER_HEADS, 2, PEER_HALF)
    s = jnp.einsum('thcd,hcnd->thcn', qh, sub_keys).astype(jnp.float32)
    s1, i1 = lax.top_k(s[:, :, 0], PEER_TOPK)
    s2, i2 = lax.top_k(s[:, :, 1], PEER_TOPK)
    cand = (s1[..., :, None] + s2[..., None, :]).reshape(t, PEER_HEADS, PEER_TOPK * PEER_TOPK)
    cand_idx = (i1[..., :, None] * N_KEYS + i2[..., None, :]).reshape(t, PEER_HEADS, PEER_TOPK * PEER_TOPK)
    top_s, pos = lax.top_k(cand, PEER_TOPK)
    eidx = jnp.take_along_axis(cand_idx, pos, axis=-1)
    g = jax.nn.softmax(top_s, axis=-1)
    nchunk = t // TOK_CHUNK

    def one_chunk(args):
        hc, ec, gc = args
        pre = jnp.einsum('cd,chkd->chk', hc, u[ec]).astype(jnp.float32)
        coef = (gc * jax.nn.gelu(pre, approximate=False)).astype(h.dtype)
        return jnp.einsum('chk,chkd->cd', coef, v[ec])

    out = lax.map(one_chunk, (h.reshape(nchunk, TOK_CHUNK, d),
                              eidx.reshape(nchunk, TOK_CHUNK, PEER_HEADS, PEER_TOPK),
                              g.reshape(nchunk, TOK_CHUNK, PEER_HEADS, PEER_TOPK)))
    return out.reshape(t, d)


def setup_inputs(seed: int = 0) -> dict:
    key = jax.random.key(seed)
    ks = jax.random.split(key, 20)
    D = D_MODEL
    nrm = jax.random.normal
    x = nrm(ks[0], (BATCH, SEQ, D), jnp.float32)
    c = nrm(ks[1], (BATCH, D), jnp.float32)
    w_ada = nrm(ks[2], (DEPTH, D, 6 * D), jnp.float32) * (0.5 * D ** -0.5)
    b_ada = nrm(ks[3], (DEPTH, 6 * D), jnp.float32) * 0.01
    norm1_g = 1.0 + 0.05 * nrm(ks[4], (DEPTH, D), jnp.float32)
    norm2_g = 1.0 + 0.05 * nrm(ks[5], (DEPTH, D), jnp.float32)
    w_in = nrm(ks[6], (DEPTH, D, IN_WIDTH), jnp.float32) * D ** -0.5
    rel_bias = nrm(ks[7], (N_BUCKETS, N_HEADS_A), jnp.float32) * 0.5
    w_proj_a = nrm(ks[8], (DEPTH, WIDTH_A, D), jnp.float32) * WIDTH_A ** -0.5
    w_proj_b = nrm(ks[9], (DEPTH, WIDTH_B, D), jnp.float32) * WIDTH_B ** -0.5
    w_out = nrm(ks[10], (DEPTH, D, D), jnp.float32) * D ** -0.5
    peer_wq = nrm(ks[11], (DEPTH, D, PEER_HEADS * PEER_QDIM), jnp.float32) * D ** -0.5
    peer_sub_keys = nrm(ks[12], (DEPTH, PEER_HEADS, 2, N_KEYS, PEER_HALF), jnp.float32) * PEER_HALF ** -0.5
    peer_u = nrm(ks[13], (DEPTH, N_EXPERTS, D), jnp.float32) * D ** -0.5
    peer_v = nrm(ks[14], (DEPTH, N_EXPERTS, D), jnp.float32) * PEER_HEADS ** -0.5
    final_g = 1.0 + 0.05 * nrm(ks[15], (D,), jnp.float32)
    return {"x": x, "c": c, "w_ada": w_ada, "b_ada": b_ada, "norm1_g": norm1_g, "norm2_g": norm2_g,
            "w_in": w_in, "rel_bias": rel_bias, "w_proj_a": w_proj_a, "w_proj_b": w_proj_b,
            "w_out": w_out, "peer_wq": peer_wq, "peer_sub_keys": peer_sub_keys, "peer_u": peer_u,
            "peer_v": peer_v, "final_g": final_g}


def reference(x, c, w_ada, b_ada, norm1_g, norm2_g, w_in, rel_bias, w_proj_a, w_proj_b, w_out,
              peer_wq, peer_sub_keys, peer_u, peer_v, final_g):
    b, s, d = x.shape
    offsets = []
    acc = 0
    for size in IN_SPLIT_SIZES[:-1]:
        acc += size
        offsets.append(acc)
    cond = jax.nn.silu(c)
    for l in range(DEPTH):
        mod = (cond @ w_ada[l] + b_ada[l]).reshape(b, 6, d)
        shift1, scale1, gate1, shift2, scale2, gate2 = [mod[:, i] for i in range(6)]

        hn = modulate(rmsnorm(x, norm1_g[l]), shift1, scale1)
        proj = hn @ w_in[l]
        qa, ka, va, qi, ki, wi, qb, kb, vb, ga, gb = jnp.split(proj, offsets, axis=-1)
        hd = (b, s, -1, HEAD_DIM)
        ya = dsa_attention(qa.reshape(hd), ka.reshape(hd), va.reshape(hd),
                           qi.reshape(b, s, IDX_HEADS, IDX_DIM), ki, wi, rel_bias)
        yb = stick_breaking_attention(qb.reshape(hd), kb.reshape(hd), vb.reshape(hd))
        merged = jax.nn.sigmoid(ga) * (ya @ w_proj_a[l]) + jax.nn.sigmoid(gb) * (yb @ w_proj_b[l])
        x = x + gate1[:, None, :] * (merged @ w_out[l])

        hn2 = modulate(rmsnorm(x, norm2_g[l]), shift2, scale2)
        f = peer_ffn(hn2.reshape(b * s, d), peer_wq[l], peer_sub_keys[l], peer_u[l], peer_v[l]).reshape(b, s, d)
        x = x + gate2[:, None, :] * f
    return rmsnorm(x, final_g)
```

```python
import math
import os
import numpy as np
import ml_dtypes
import concourse.bass as bass
import concourse.mybir as mybir
from concourse.bass_utils import run_bass_kernel_spmd
from contextlib import ExitStack

F32 = mybir.dt.float32
BF16 = mybir.dt.bfloat16
AF = mybir.ActivationFunctionType
ALU = mybir.AluOpType
AX = mybir.AxisListType
ENGS = ("sync", "scalar", "vector", "gpsimd", "tensor")
NSLOT = 8
SB_BASE = 16512
SB_LIM = 16512 + 208000
NEG = -1.0e30


class Buf:
    __slots__ = ("name", "w", "r")

    def __init__(self, name=""):
        self.name = name
        self.w = None
        self.r = {}


class Rec:
    def __init__(self, nc, stack):
        self.nc = nc
        self.sem = {}
        for e in ENGS:
            self.sem[("c", e)] = stack.enter_context(nc.semaphore("c_" + e))
        for q in ("sync", "scalar", "gpsimd"):
            for s in range(NSLOT):
                self.sem[("d", q, s)] = stack.enter_context(nc.semaphore(f"d_{q}_{s}"))
        self.ops = {e: [] for e in ENGS}
        self.cnt = {e: 0 for e in ENGS}
        self.dcnt = {q: 0 for q in ("sync", "scalar", "gpsimd")}
        self.seen = {e: {} for e in ENGS}
        self.sb_off = SB_BASE
        self.sb_hi = SB_BASE
        self.uid = 0

    def sb(self, shape, dtype, name=None):
        self.uid += 1
        nbytes = int(np.prod(shape[1:])) * (4 if dtype == F32 else 2)
        nbytes = (nbytes + 63) // 64 * 64
        off = self.sb_off
        self.sb_off += nbytes
        self.sb_hi = max(self.sb_hi, self.sb_off)
        assert self.sb_off <= SB_LIM, f"SBUF overflow {self.sb_off}"
        return self.nc.alloc_sbuf_tensor_at(f"{name or 't'}_{self.uid}", list(shape), dtype, offset=off)

    def mark(self):
        return self.sb_off

    def release(self, m):
        self.sb_off = m

    def op(self, eng, fn, reads=(), writes=(), dma=False):
        waits = {}

        def need(ev):
            if ev is None:
                return
            key, val = ev
            if key == ("c", "tensor") and eng == "tensor" and not dma:
                return
            if waits.get(key, 0) < val:
                waits[key] = val

        for b in reads:
            need(b.w)
        for b in writes:
            need(b.w)
            for k, v in b.r.items():
                need((k, v))
        wl = []
        seen = self.seen[eng]
        for key, val in waits.items():
            if seen.get(key, 0) >= val:
                continue
            seen[key] = val
            wl.append((key, val))
        if dma:
            n = self.dcnt[eng]
            self.dcnt[eng] += 1
            slot = n % NSLOT
            val = 16 * (n // NSLOT + 1)
            key = ("d", eng, slot)
            if n >= NSLOT and seen.get(key, 0) < val - 16:
                wl.append((key, val - 16))
                seen[key] = val - 16
            ev = (key, val)
            inc = 16
        else:
            self.cnt[eng] += 1
            ev = (("c", eng), self.cnt[eng])
            inc = 1
        self.ops[eng].append((fn, wl, ev, inc))
        for b in reads:
            if b.r.get(ev[0], 0) < ev[1]:
                b.r[ev[0]] = ev[1]
        for b in writes:
            b.w = ev
            b.r = {}
        return ev

    def barrier(self):
        evs = []
        for e in ENGS:
            if self.cnt[e]:
                evs.append((("c", e), self.cnt[e]))
        for q, n in self.dcnt.items():
            for s in range(NSLOT):
                if n > s:
                    k = (n - 1 - s) // NSLOT + 1
                    evs.append((("d", q, s), 16 * k))
        for e in ENGS:
            wl = []
            for key, val in evs:
                if key == ("c", e):
                    continue
                if self.seen[e].get(key, 0) < val:
                    self.seen[e][key] = val
                    wl.append((key, val))
            if wl:
                self.ops[e].append((None, wl, None, 0))

    def emit(self):
        nc = self.nc
        with nc.Block() as block:
            for e in ENGS:
                ops = self.ops[e]
                if not ops:
                    continue

                def body(eng, ops=ops):
                    for fn, wl, ev, inc in ops:
                        for key, val in wl:
                            eng.wait_ge(self.sem[key], val)
                        if fn is not None:
                            ins = fn(eng)
                            ins.then_inc(self.sem[ev[0]], inc)

                getattr(block, e)(body)
        self.ops = {e: [] for e in ENGS}


class Rot:
    def __init__(self, items):
        self.items = [(t, Buf()) for t in items]
        self.i = 0

    def next(self):
        it = self.items[self.i % len(self.items)]
        self.i += 1
        return it


def build(stop_after="E", dbg=False, NG=16):
    nc = bass.Bass("TRN2", target_bir_lowering=False)
    S = 512 * NG
    NQ = 128 * NG
    NT = 4 * NG

    def din(name, shape, dt=F32):
        return nc.dram_tensor(name, list(shape), dt, kind="ExternalInput").ap()

    def dscr(name, shape, dt):
        return nc.dram_tensor(name, list(shape), dt).ap()

    x_d = din("x", [S, 1024])
    xo_d = din("xo", [NQ, 1024])
    cT_d = din("cT", [128, 8])
    wada_d = din("w_ada", [1024, 6144])
    bada_d = din("b_ada", [1, 6144])
    g1_d = din("g1", [1, 1024])
    g2_d = din("g2", [1, 1024])
    gf_d = din("gf", [1, 1024])
    wK_d = din("wK", [1024, 1088])
    wV_d = din("wV", [1024, 1024])
    wQ_d = din("wQ", [1024, 1536])
    wG_d = din("wG", [1024, 2048])
    wWi_d = din("wWi", [1024, 8])
    bias_d = din("biasT", [6, 128, 8, 128])
    const_d = din("consts", [15, 128, 128])
    wpa_d = din("wpa", [512, 1024])
    wpb_d = din("wpb", [512, 1024])
    wout_d = din("wout", [1024, 1024])
    wq_d = din("wq", [1024, 2048])
    skT_d = din("skT", [128, 16, 128])
    uT_d = din("uT", [1024, 16384])
    v_d = din("pv", [16384, 1024])
    out_d = nc.dram_tensor("out", [NQ, 1024], F32, kind="ExternalOutput").ap()
    dbg_d = {}
    if dbg:
        dbg_d["yb"] = nc.dram_tensor("dbg_yb", [128, NG, 512], F32, kind="ExternalOutput").ap()
        dbg_d["ya"] = nc.dram_tensor("dbg_ya", [128, NG, 512], F32, kind="ExternalOutput").ap()
        dbg_d["mod"] = nc.dram_tensor("dbg_mod", [128, 6144], F32, kind="ExternalOutput").ap()
        dbg_d["x2"] = nc.dram_tensor("dbg_x2", [NQ, 1024], F32, kind="ExternalOutput").ap()

    kaT_s = dscr("kaT_s", [4, 128, S], BF16)
    kbT_s = dscr("kbT_s", [4, 128, S], BF16)
    kiT_s = dscr("kiT_s", [64, S], BF16)
    va_s = dscr("va_s", [S, 512], BF16)
    vb_s = dscr("vb_s", [S, 512], BF16)
    gT_s = dscr("gT_s", [16, 128, NQ], BF16)
    q_s = dscr("q_s", [24, 128, NQ], BF16)
    maskT_s = dscr("maskT_s", [NG, NT, 128, 128], BF16)
    x2_s = dscr("x2_s", [NQ, 1024], F32)

    with ExitStack() as st:
        R = Rec(nc, st)
        op = R.op
        PS = [nc.alloc_psum_tensor(f"psb{i}", [128, 512], F32) for i in range(8)]

        consts = R.sb([128, 15, 128], F32, "consts")
        bconst = Buf()
        ident_f = consts[:, 0, :]
        tri_f = consts[:, 1, :]
        ones_f = consts[:, 2, :]
        msb_f = consts[:, 3:7, :]
        caus01 = consts[:, 7:11, :]
        causneg = consts[:, 11:15, :]
        ident_b = R.sb([128, 128], BF16, "identb")
        msb_b = R.sb([128, 4, 128], BF16, "msbb")
        mod_bc = R.sb([128, 6144], F32, "mod")
        bmod = Buf()
        gmod1 = R.sb([128, 1024], F32, "gmod1")
        gmod2 = R.sb([128, 1024], F32, "gmod2")
        gf_bc = R.sb([128, 1024], F32, "gf")
        bgm = Buf()
        wi_sb = R.sb([128, NG, 8], F32, "wi")
        bwi = Buf()
        shift1 = mod_bc[:, 0:1024]
        gate1 = mod_bc[:, 2048:3072]
        shift2 = mod_bc[:, 3072:4096]
        gate2 = mod_bc[:, 5120:6144]

        op("sync", lambda e: e.dma_start(out=consts[:], in_=const_d.rearrange("k p n -> p k n")), writes=[bconst], dma=True)
        op("vector", lambda e: e.tensor_copy(out=ident_b[:], in_=ident_f), reads=[bconst], writes=[bconst])
        op("vector", lambda e: e.tensor_copy(out=msb_b[:], in_=msb_f), reads=[bconst], writes=[bconst])

        mk0 = R.mark()
        cT = R.sb([128, 8], F32)
        bc = Buf()
        condrep = R.sb([128, 8, 128], F32)
        bada_bc = R.sb([128, 6144], F32)
        bb = Buf()
        g_bc = R.sb([128, 2, 1024], F32)
        bg = Buf()
        wa_rot = Rot([R.sb([128, 8, 512], F32) for _ in range(2)])
        op("sync", lambda e: e.dma_start(out=cT[:], in_=cT_d), writes=[bc], dma=True)
        op("sync", lambda e: e.dma_start(out=bada_bc[:], in_=bada_d.partition_broadcast(128)), writes=[bb], dma=True)
        op("sync", lambda e: e.dma_start(out=g_bc[:, 0, :], in_=g1_d.partition_broadcast(128)), writes=[bg], dma=True)
        op("sync", lambda e: e.dma_start(out=g_bc[:, 1, :], in_=g2_d.partition_broadcast(128)), writes=[bg], dma=True)
        op("sync", lambda e: e.dma_start(out=gf_bc[:], in_=gf_d.partition_broadcast(128)), writes=[bgm], dma=True)
        op("scalar", lambda e: e.activation(out=cT[:], in_=cT[:], func=AF.Silu), reads=[bc], writes=[bc])
        for c in range(8):
            op("vector", lambda e, c=c: e.tensor_scalar(out=condrep[:, c, :], in0=ones_f, scalar1=cT[:, c:c + 1], scalar2=None, op0=ALU.mult),
               reads=[bc, bconst], writes=[bc])
        ps0_rot = Rot([PS[0], PS[1]])
        for nn in range(12):
            wt, bw = wa_rot.next()
            op("sync", lambda e, wt=wt, nn=nn: e.dma_start(out=wt[:], in_=wada_d[:, nn * 512:(nn + 1) * 512].rearrange("(c p) n -> p c n", p=128)),
               writes=[bw], dma=True)
            ps, bps = ps0_rot.next()
            for c in range(8):
                op("tensor", lambda e, ps=ps, wt=wt, c=c: e.matmul(ps[:], lhsT=condrep[:, c, :], rhs=wt[:, c, :], start=(c == 0), stop=(c == 7)),
                   reads=[bc, bw], writes=[bps])
            op("vector", lambda e, ps=ps, nn=nn: e.tensor_tensor(out=mod_bc[:, nn * 512:(nn + 1) * 512], in0=ps[:], in1=bada_bc[:, nn * 512:(nn + 1) * 512], op=ALU.add),
               reads=[bps, bb], writes=[bmod])
        op("vector", lambda e: e.scalar_tensor_tensor(out=gmod1[:], in0=mod_bc[:, 1024:2048], scalar=1.0, in1=g_bc[:, 0, :], op0=ALU.add, op1=ALU.mult),
           reads=[bmod, bg], writes=[bgm])
        op("vector", lambda e: e.scalar_tensor_tensor(out=gmod2[:], in0=mod_bc[:, 4096:5120], scalar=1.0, in1=g_bc[:, 1, :], op0=ALU.add, op1=ALU.mult),
           reads=[bmod, bg], writes=[bgm])
        if dbg:
            op("sync", lambda e: e.dma_start(out=dbg_d["mod"], in_=mod_bc[:]), reads=[bmod], dma=True)
        R.barrier()
        R.release(mk0)

        mkA = R.mark()
        wK = R.sb([128, 8, 1088], BF16)
        wV = R.sb([128, 8, 1024], BF16)
        bw = Buf()
        for wt, wd in ((wK, wK_d), (wV, wV_d)):
            op("gpsimd", lambda e, wt=wt, wd=wd: e.dma_start(out=wt[:], in_=wd.rearrange("(c p) n -> p c n", p=128)), writes=[bw], dma=True)
        kst_rot = Rot([R.sb([128, 9, 512], BF16) for _ in range(1)])
        vst_rot = Rot([R.sb([128, 4, 1024], BF16) for _ in range(1)])
        psT_rot = Rot([PS[0], PS[1]])
        psM_rot = Rot([PS[2], PS[3], PS[4], PS[5], PS[6], PS[7]])
        W = {}

        def alloc_norm_work():
            W["xt"] = Rot([R.sb([128, 4, 1024], F32) for _ in range(1)])
            W["hn"] = Rot([R.sb([128, 4, 1024], BF16) for _ in range(1)])
            W["hnT"] = Rot([R.sb([128, 8, 512], BF16) for _ in range(2)])
            W["junk"] = R.sb([128, 1024], BF16)
            W["ss"] = Rot([R.sb([128, 4], F32) for _ in range(2)])

        alloc_norm_work()
        bjunk = Buf()
        evac_i = [0]

        def evac(out_ap, in_ap, reads, writes, func=None, scale=None):
            if func is not None:
                op("scalar", lambda e: e.activation(out=out_ap, in_=in_ap, func=func), reads=reads, writes=writes)
                return
            evac_i[0] += 1
            if scale is not None:
                if evac_i[0] % 2:
                    op("scalar", lambda e: e.mul(out=out_ap, in_=in_ap, mul=scale), reads=reads, writes=writes)
                else:
                    op("vector", lambda e: e.tensor_scalar(out=out_ap, in0=in_ap, scalar1=scale, scalar2=None, op0=ALU.mult), reads=reads, writes=writes)
                return
            if evac_i[0] % 2:
                op("scalar", lambda e: e.copy(out=out_ap, in_=in_ap), reads=reads, writes=writes)
            else:
                op("vector", lambda e: e.tensor_copy(out=out_ap, in_=in_ap), reads=reads, writes=writes)

        def norm_group(src_ap, nt):
            xt, bx = W["xt"].next()
            hn, bhn = W["hn"].next()
            hnT, bhnT = W["hnT"].next()
            ss, bss = W["ss"].next()
            sq_junk = W["junk"]
            op("sync", lambda e: e.dma_start(out=xt[:, 0:nt, :], in_=src_ap.rearrange("(tt p) d -> p tt d", p=128)), writes=[bx], dma=True)
            for tt in range(nt):
                op("scalar", lambda e, tt=tt: e.activation(out=sq_junk[:], in_=xt[:, tt, :], func=AF.Square, accum_out=ss[:, tt:tt + 1]),
                   reads=[bx], writes=[bjunk, bss])
            op("vector", lambda e: e.tensor_scalar(out=ss[:, 0:nt], in0=ss[:, 0:nt], scalar1=1.0 / 1024, scalar2=1e-6, op0=ALU.mult, op1=ALU.add), reads=[bss], writes=[bss])
            op("scalar", lambda e: e.sqrt(out=ss[:, 0:nt], in_=ss[:, 0:nt]), reads=[bss], writes=[bss])
            op("vector", lambda e: e.reciprocal(out=ss[:, 0:nt], in_=ss[:, 0:nt]), reads=[bss], writes=[bss])
            for tt in range(nt):
                op("vector", lambda e, tt=tt: e.scalar_tensor_tensor(out=xt[:, tt, :], in0=xt[:, tt, :], scalar=ss[:, tt:tt + 1], in1=gmod1[:], op0=ALU.mult, op1=ALU.mult),
                   reads=[bx, bss, bgm], writes=[bx])
                op("gpsimd", lambda e, tt=tt: e.tensor_tensor(out=hn[:, tt, :], in0=xt[:, tt, :], in1=shift1, op=ALU.add),
                   reads=[bx, bmod], writes=[bhn])
            for tt in range(nt):
                pst, bpst = psT_rot.next()
                pstb = pst[:].bitcast(BF16)
                for c in range(8):
                    op("tensor", lambda e, pstb=pstb, tt=tt, c=c: e.transpose(out=pstb[:, c * 128:(c + 1) * 128], in_=hn[:, tt, c * 128:(c + 1) * 128], identity=ident_b[:]),
                       reads=[bhn, bconst], writes=[bpst])
                evac(hnT[:, :, tt * 128:(tt + 1) * 128], pstb.rearrange("p (c t) -> p c t", c=8), [bpst], [bhnT])
            return hnT, bhnT

        for m in range(NG):
            hnT, bhnT = norm_group(x_d[m * 512:(m + 1) * 512, :], 4)
            kst, bkst = kst_rot.next()
            vst, bvst = vst_rot.next()
            for o in range(9):
                wdt = 128 if o < 8 else 64
                ps, bps = psM_rot.next()
                for c in range(8):
                    op("tensor", lambda e, ps=ps, hnT=hnT, o=o, c=c, wdt=wdt: e.matmul(ps[0:wdt, :], lhsT=wK[:, c, o * 128:o * 128 + wdt], rhs=hnT[:, c, :], start=(c == 0), stop=(c == 7)),
                       reads=[bw, bhnT], writes=[bps])
                evac(kst[0:wdt, o, :], ps[0:wdt, :], [bps], [bkst])
            op("sync", lambda e, kst=kst, m=m: e.dma_start(out=kaT_s[:, :, m * 512:(m + 1) * 512].rearrange("o p t -> p o t"), in_=kst[:, 0:4, :]), reads=[bkst], dma=True)
            op("sync", lambda e, kst=kst, m=m: e.dma_start(out=kbT_s[:, :, m * 512:(m + 1) * 512].rearrange("o p t -> p o t"), in_=kst[:, 4:8, :]), reads=[bkst], dma=True)
            op("sync", lambda e, kst=kst, m=m: e.dma_start(out=kiT_s[:, m * 512:(m + 1) * 512], in_=kst[0:64, 8, :]), reads=[bkst], dma=True)
            for tt in range(4):
                for half in range(2):
                    ps, bps = psM_rot.next()
                    for c in range(8):
                        op("tensor", lambda e, ps=ps, hnT=hnT, tt=tt, half=half, c=c: e.matmul(ps[:], lhsT=hnT[:, c, tt * 128:(tt + 1) * 128], rhs=wV[:, c, half * 512:(half + 1) * 512], start=(c == 0), stop=(c == 7)),
                           reads=[bw, bhnT], writes=[bps])
                    evac(vst[:, tt, half * 512:(half + 1) * 512], ps[:], [bps], [bvst])
            op("sync", lambda e, vst=vst, m=m: e.dma_start(out=va_s[m * 512:(m + 1) * 512, :].rearrange("(tt p) c -> p tt c", p=128), in_=vst[:, :, 0:512]), reads=[bvst], dma=True)
            op("sync", lambda e, vst=vst, m=m: e.dma_start(out=vb_s[m * 512:(m + 1) * 512, :].rearrange("(tt p) c -> p tt c", p=128), in_=vst[:, :, 512:1024]), reads=[bvst], dma=True)

        R.barrier()
        R.release(mkA)
        wQ = R.sb([128, 8, 1536], BF16)
        wG = R.sb([128, 8, 2048], BF16)
        wWi = R.sb([128, 8, 8], BF16)
        bw = Buf()
        for wt, wd in ((wQ, wQ_d), (wG, wG_d), (wWi, wWi_d)):
            op("gpsimd", lambda e, wt=wt, wd=wd: e.dma_start(out=wt[:], in_=wd.rearrange("(c p) n -> p c n", p=128)), writes=[bw], dma=True)
        gst_rot = Rot([R.sb([128, 512], BF16) for _ in range(3)])
        zt = R.sb([128, 512], BF16)
        bzt = Buf()
        op("vector", lambda e: e.memset(zt[:], 0.0), writes=[bzt])
        alloc_norm_work()
        for mm in range((NG + 3) // 4):
            nt = min(4, NG - 4 * mm)
            NW = nt * 128
            hnT, bhnT = norm_group(xo_d[mm * 512:mm * 512 + NW, :], nt)
            for og in range(3):
                for oo in range(4):
                    o = og * 4 + oo
                    ps, bps = psM_rot.next()
                    for c in range(8):
                        op("tensor", lambda e, ps=ps, hnT=hnT, o=o, c=c, NW=NW: e.matmul(ps[:, 0:NW], lhsT=wQ[:, c, o * 128:(o + 1) * 128], rhs=hnT[:, c, 0:NW], start=(c == 0), stop=(c == 7)),
                           reads=[bw, bhnT], writes=[bps])
                    gst, bgst = gst_rot.next()
                    evac(gst[:, 0:NW], ps[:, 0:NW], [bps], [bgst], scale=(0.125 if og < 2 else None))
                    cs = slice(mm * 512, mm * 512 + NW)
                    op("sync", lambda e, gst=gst, o=o, cs=cs, NW=NW: e.dma_start(out=q_s[2 * o, 0:64, cs], in_=gst[0:64, 0:NW]), reads=[bgst], dma=True)
                    op("sync", lambda e, o=o, cs=cs, NW=NW: e.dma_start(out=q_s[2 * o, 64:128, cs], in_=zt[64:128, 0:NW]), reads=[bzt], dma=True)
                    op("sync", lambda e, gst=gst, o=o, cs=cs, NW=NW: e.dma_start(out=q_s[2 * o + 1, 64:128, cs], in_=gst[64:128, 0:NW]), reads=[bgst], dma=True)
                    op("sync", lambda e, o=o, cs=cs, NW=NW: e.dma_start(out=q_s[2 * o + 1, 0:64, cs], in_=zt[0:64, 0:NW]), reads=[bzt], dma=True)
            for o in range(16):
                ps, bps = psM_rot.next()
                gst, bgst = gst_rot.next()
                for c in range(8):
                    op("tensor", lambda e, ps=ps, hnT=hnT, o=o, c=c, NW=NW: e.matmul(ps[:, 0:NW], lhsT=wG[:, c, o * 128:(o + 1) * 128], rhs=hnT[:, c, 0:NW], start=(c == 0), stop=(c == 7)),
                       reads=[bw, bhnT], writes=[bps])
                evac(gst[:, 0:NW], ps[:, 0:NW], [bps], [bgst], func=AF.Sigmoid)
                op("sync", lambda e, gst=gst, o=o, mm=mm, NW=NW: e.dma_start(out=gT_s[o, :, mm * 512:mm * 512 + NW], in_=gst[:, 0:NW]), reads=[bgst], dma=True)
            for tt in range(nt):
                ps, bps = psM_rot.next()
                for c in range(8):
                    op("tensor", lambda e, ps=ps, hnT=hnT, c=c, tt=tt: e.matmul(ps[:, 0:8], lhsT=hnT[:, c, tt * 128:(tt + 1) * 128], rhs=wWi[:, c, :], start=(c == 0), stop=(c == 7)),
                       reads=[bw, bhnT], writes=[bps])
                op("vector", lambda e, ps=ps, mm=mm, tt=tt: e.tensor_copy(out=wi_sb[:, mm * 4 + tt, :], in_=ps[:, 0:8]), reads=[bps], writes=[bwi])
        R.barrier()
        R.release(mkA)
        mkY = R.mark()
        yb_sb = R.sb([128, NG, 512], BF16, "yb")
        ya_sb = R.sb([128, NG, 512], BF16, "ya")
        bya = Buf()
        byb = Buf()

        if stop_after >= "B":
            mkB = R.mark()
            kT = R.sb([128, 2, S], BF16)
            vg = R.sb([128, NT, 256], BF16)
            bkv = Buf()
            qbT = R.sb([128, 4, NQ], BF16)
            bq = Buf()
            e_rot = Rot([R.sb([128, 512], F32) for _ in range(2)])
            sp_rot = Rot([R.sb([128, 512], F32) for _ in range(2)])
            arg_rot = Rot([R.sb([128, 512], F32) for _ in range(2)])
            A_rot = Rot([R.sb([128, 512], BF16) for _ in range(3)])
            carry_rot = Rot([R.sb([128, 512], F32) for _ in range(2)])
            z_rot = Rot([PS[0], PS[1], PS[2]])
            c_rot = Rot([PS[3], PS[4], PS[5]])
            y_rot = Rot([PS[6], PS[7]])
            for g in range(2):
                op("sync", lambda e, g=g: e.dma_start(out=qbT[:], in_=q_s[8 + 4 * g:12 + 4 * g].rearrange("o p t -> p o t")), writes=[bq], dma=True)
                op("sync", lambda e, g=g: e.dma_start(out=kT[:], in_=kbT_s[2 * g:2 * g + 2].rearrange("o p t -> p o t")), writes=[bkv], dma=True)
                for n8 in range(0, NT, 8):
                    ne = min(NT, n8 + 8)
                    op("sync", lambda e, g=g, n8=n8, ne=ne: e.dma_start(out=vg[:, n8:ne, :], in_=vb_s[n8 * 128:ne * 128, g * 256:(g + 1) * 256].rearrange("(n p) c -> p n c", p=128)), writes=[bkv], dma=True)
                for m in range(NG):
                    jtop = 4 * m + 3
                    carry, bcar = carry_rot.next()
                    yps, byps = y_rot.next()
                    op("gpsimd", lambda e, carry=carry: e.memset(carry[:], 0.0), writes=[bcar])
                    for jb in range(jtop, -1, -1):
                        diag = jb >= 4 * m
                        r = jb - 4 * m
                        zps, bz = z_rot.next()
                        cps, bcp = c_rot.next()
                        et, be = e_rot.next()
                        sp, bsp = sp_rot.next()
                        arg, barg = arg_rot.next()
                        At, bA = A_rot.next()
                        for h in range(4):
                            pr = slice((h % 2) * 64, (h % 2) * 64 + 64)
                            op("tensor", lambda e, zps=zps, h=h, pr=pr, jb=jb, m=m, g=g: e.matmul(zps[:, h * 128:(h + 1) * 128], lhsT=kT[:, h // 2, jb * 128:(jb + 1) * 128], rhs=qbT[:, h, m * 128:(m + 1) * 128], start=(h == 0), stop=False, skip_group_check=True),
                               reads=[bkv, bq], writes=[bz])
                        op("scalar", lambda e, et=et, zps=zps: e.activation(out=et[:], in_=zps[:], func=AF.Exp), reads=[bz], writes=[be])
                        op("scalar", lambda e, et=et, sp=sp: e.activation(out=sp[:], in_=et[:], func=AF.Ln, bias=1.0), reads=[be], writes=[bsp])
                        if diag:
                            op("vector", lambda e, sp=sp, r=r: e.tensor_tensor(out=sp[:].rearrange("p (h t) -> p h t", h=4), in0=sp[:].rearrange("p (h t) -> p h t", h=4), in1=msb_f[:, r, :].unsqueeze(1).to_broadcast([128, 4, 128]), op=ALU.mult),
                               reads=[bsp, bconst], writes=[bsp])
                        op("tensor", lambda e, zps=zps, sp=sp: e.matmul(zps[:], lhsT=tri_f, rhs=sp[:], start=False, stop=True, skip_group_check=True), reads=[bsp, bconst], writes=[bz])
                        op("tensor", lambda e, cps=cps, sp=sp: e.matmul(cps[:], lhsT=ones_f, rhs=sp[:], start=True, stop=True), reads=[bsp, bconst], writes=[bcp])
                        op("vector", lambda e, arg=arg, zps=zps, carry=carry: e.tensor_tensor(out=arg[:], in0=zps[:], in1=carry[:], op=ALU.subtract), reads=[bz, bcar], writes=[barg])
                        op("vector", lambda e, cps=cps, carry=carry: e.tensor_tensor(out=carry[:], in0=cps[:], in1=carry[:], op=ALU.add), reads=[bcp, bcar], writes=[bcar])
                        op("scalar", lambda e, At=At, arg=arg: e.activation(out=At[:], in_=arg[:], func=AF.Exp), reads=[barg], writes=[bA])
                        if diag:
                            op("gpsimd", lambda e, At=At, r=r: e.tensor_tensor(out=At[:].rearrange("p (h t) -> p h t", h=4), in0=At[:].rearrange("p (h t) -> p h t", h=4), in1=msb_b[:, r, :].unsqueeze(1).to_broadcast([128, 4, 128]), op=ALU.mult),
                               reads=[bA, bconst], writes=[bA])
                        for h in range(4):
                            op("tensor", lambda e, yps=yps, At=At, h=h, jb=jb, jtop=jtop: e.matmul(yps[:, h * 64:(h + 1) * 64], lhsT=At[:, h * 128:(h + 1) * 128], rhs=vg[:, jb, h * 64:(h + 1) * 64], start=(jb == jtop and h == 0), stop=(jb == 0), skip_group_check=True),
                               reads=[bA, bkv], writes=[byps])
                    op("vector", lambda e, yps=yps, m=m, g=g: e.tensor_copy(out=yb_sb[:, m, g * 256:(g + 1) * 256], in_=yps[:, 0:256]), reads=[byps], writes=[byb])
            R.barrier()
            R.release(mkB)
            if dbg:
                ybf = R.sb([128, NG, 512], F32)
                bt = Buf()
                op("vector", lambda e: e.tensor_copy(out=ybf[:], in_=yb_sb[:]), reads=[byb], writes=[bt])
                op("sync", lambda e: e.dma_start(out=dbg_d["yb"], in_=ybf[:]), reads=[bt], dma=True)
                R.barrier()
                R.release(mkB)

        if stop_after >= "C":
            mkC = R.mark()
            kiT2 = R.sb([128, S], BF16)
            bki = Buf()
            op("sync", lambda e: e.dma_start(out=kiT2[0:64, :], in_=kiT_s), writes=[bki], dma=True)
            op("sync", lambda e: e.dma_start(out=kiT2[64:128, :], in_=kiT_s), writes=[bki], dma=True)
            score = R.sb([128, S], F32)
            bsc = Buf()
            work = R.sb([128, S], F32)
            bwk = Buf()
            mask01 = R.sb([128, S], BF16)
            bmk = Buf()
            qi_rot = Rot([R.sb([128, 8, 128], BF16) for _ in range(2)])
            r_rot = Rot([R.sb([128, 512], F32) for _ in range(3)])
            m8_rot = Rot([R.sb([128, 8], F32) for _ in range(2)])
            thr = R.sb([128, 1], F32)
            bthr = Buf()
            mst_rot = Rot([R.sb([128, 8, 128], BF16) for _ in range(2)])
            sc_rot = Rot([PS[0], PS[1], PS[2], PS[3]])
            tp_rot = Rot([PS[4], PS[5]])
            for m in range(NG):
                nblk = 4 * m + 4
                nk = nblk * 128
                qi_t, bqi = qi_rot.next()
                op("sync", lambda e, qi_t=qi_t, m=m: e.dma_start(out=qi_t[:], in_=q_s[16:24, :, m * 128:(m + 1) * 128].rearrange("o p t -> p o t")), writes=[bqi], dma=True)
                for ck in range(m + 1):
                    cs = slice(ck * 512, (ck + 1) * 512)
                    for h in range(8):
                        ps, bps = sc_rot.next()
                        rt, brt = r_rot.next()
                        op("tensor", lambda e, ps=ps, qi_t=qi_t, h=h, cs=cs: e.matmul(ps[:], lhsT=qi_t[:, h, :], rhs=kiT2[:, cs], start=True, stop=True), reads=[bqi, bki], writes=[bps])
                        op("scalar", lambda e, ps=ps, rt=rt: e.activation(out=rt[:], in_=ps[:], func=AF.Relu), reads=[bps], writes=[brt])
                        if h == 0:
                            op("vector", lambda e, rt=rt, cs=cs, m=m: e.tensor_scalar(out=score[:, cs], in0=rt[:], scalar1=wi_sb[:, m, 0:1], scalar2=None, op0=ALU.mult), reads=[brt, bwi], writes=[bsc])
                        else:
                            op("vector", lambda e, rt=rt, cs=cs, m=m, h=h: e.scalar_tensor_tensor(out=score[:, cs], in0=rt[:], scalar=wi_sb[:, m, h:h + 1], in1=score[:, cs], op0=ALU.mult, op1=ALU.add), reads=[brt, bwi, bsc], writes=[bsc])
                last = slice(4 * m * 128, nk)
                op("vector", lambda e, last=last: e.tensor_tensor(out=score[:, last].rearrange("p (r s) -> p r s", r=4), in0=score[:, last].rearrange("p (r s) -> p r s", r=4), in1=caus01, op=ALU.mult), reads=[bsc, bconst], writes=[bsc])
                op("vector", lambda e, last=last: e.tensor_tensor(out=score[:, last].rearrange("p (r s) -> p r s", r=4), in0=score[:, last].rearrange("p (r s) -> p r s", r=4), in1=causneg, op=ALU.add), reads=[bsc, bconst], writes=[bsc])
                for it in range(32):
                    m8, bm8 = m8_rot.next()
                    src = score if it == 0 else work
                    bsrc = bsc if it == 0 else bwk
                    op("vector", lambda e, m8=m8, src=src, nk=nk: e.max(out=m8[:], in_=src[:, 0:nk]), reads=[bsrc], writes=[bm8])
                    if it < 31:
                        op("vector", lambda e, m8=m8, src=src, nk=nk: e.match_replace(out=work[:, 0:nk], in_to_replace=m8[:], in_values=src[:, 0:nk], imm_value=NEG), reads=[bsrc, bm8], writes=[bwk])
                op("vector", lambda e, m8=m8: e.tensor_scalar(out=thr[:], in0=m8[:, 7:8], scalar1=-1.0e29, scalar2=None, op0=ALU.max), reads=[bm8], writes=[bthr])
                op("vector", lambda e, nk=nk: e.tensor_scalar(out=mask01[:, 0:nk], in0=score[:, 0:nk], scalar1=thr[:, 0:1], scalar2=None, op0=ALU.is_ge), reads=[bsc, bthr], writes=[bmk])
                for b0 in range(0, nblk, 8):
                    nb = min(8, nblk - b0)
                    tp, btp = tp_rot.next()
                    tpb = tp[:].bitcast(BF16)
                    mst, bmst = mst_rot.next()
                    for bb_ in range(nb):
                        op("tensor", lambda e, tpb=tpb, bb_=bb_, b0=b0: e.transpose(out=tpb[:, bb_ * 128:(bb_ + 1) * 128], in_=mask01[:, (b0 + bb_) * 128:(b0 + bb_ + 1) * 128], identity=ident_b[:]), reads=[bmk, bconst], writes=[btp])
                    op("scalar", lambda e, tpb=tpb, mst=mst, nb=nb: e.copy(out=mst[:, 0:nb, :], in_=tpb[:, 0:nb * 128].rearrange("p (n t) -> p n t", n=nb)), reads=[btp], writes=[bmst])
                    op("sync", lambda e, mst=mst, m=m, b0=b0, nb=nb: e.dma_start(out=maskT_s[m, b0:b0 + nb].rearrange("n p t -> p n t"), in_=mst[:, 0:nb, :]), reads=[bmst], dma=True)
            R.barrier()
            R.release(mkC)

            kT = R.sb([128, 2, S], BF16)
            va_g = R.sb([128, NT, 4, 65], BF16)
            bkv = Buf()
            vtmp_rot = Rot([R.sb([128, 8, 256], BF16) for _ in range(2)])
            bt = R.sb([128, 5, 4, 128], F32)
            bfar = R.sb([128, 4, 128], F32)
            bbt = Buf()
            maskT = R.sb([128, NT, 128], BF16)
            bmT = Buf()
            qa_rot = Rot([R.sb([128, 4, 128], BF16) for _ in range(2)])
            P_rot = Rot([R.sb([128, 512], BF16) for _ in range(3)])
            Pm_rot = Rot([R.sb([128, 512], BF16) for _ in range(3)])
            den = R.sb([128, 4], F32)
            bden = Buf()
            L_rot = Rot([PS[0], PS[1], PS[2], PS[3]])
            y_rot = Rot([PS[6], PS[7]])
            for g in range(2):
                op("sync", lambda e, g=g: e.dma_start(out=kT[:], in_=kaT_s[2 * g:2 * g + 2].rearrange("o p t -> p o t")), writes=[bkv], dma=True)
                op("gpsimd", lambda e: e.memset(va_g[:, :, :, 64:65], 1.0), writes=[bkv])
                for n8 in range(0, NT, 8):
                    ne = min(NT, n8 + 8)
                    vt, bvt = vtmp_rot.next()
                    op("sync", lambda e, g=g, n8=n8, ne=ne, vt=vt: e.dma_start(out=vt[:, 0:ne - n8, :], in_=va_s[n8 * 128:ne * 128, g * 256:(g + 1) * 256].rearrange("(n p) c -> p n c", p=128)), writes=[bvt], dma=True)
                    op("gpsimd", lambda e, n8=n8, ne=ne, vt=vt: e.tensor_copy(out=va_g[:, n8:ne, :, 0:64], in_=vt[:, 0:ne - n8, :].rearrange("p n (h d) -> p n h d", h=4)), reads=[bvt], writes=[bkv])
                op("sync", lambda e, g=g: e.dma_start(out=bt[:], in_=bias_d[0:5, :, 4 * g:4 * g + 4, :].rearrange("r p h t -> p r h t")), writes=[bbt], dma=True)
                op("sync", lambda e, g=g: e.dma_start(out=bfar[:], in_=bias_d[5, :, 4 * g:4 * g + 4, :]), writes=[bbt], dma=True)
                for r5 in range(5):
                    op("vector", lambda e, r5=r5: e.tensor_tensor(out=bt[:, r5, :, :], in0=bt[:, r5, :, :], in1=bfar[:], op=ALU.subtract), reads=[bbt], writes=[bbt])
                for m in range(NG):
                    jtop = 4 * m + 3
                    nblk = jtop + 1
                    qa_t, bqa = qa_rot.next()
                    yps, byps = y_rot.next()
                    op("sync", lambda e, qa_t=qa_t, m=m, g=g: e.dma_start(out=qa_t[:], in_=q_s[4 * g:4 * g + 4, :, m * 128:(m + 1) * 128].rearrange("o p t -> p o t")), writes=[bqa], dma=True)
                    for b0 in range(0, nblk, 8):
                        nb = min(8, nblk - b0)
                        op("sync", lambda e, m=m, b0=b0, nb=nb: e.dma_start(out=maskT[:, b0:b0 + nb, :], in_=maskT_s[m, b0:b0 + nb].rearrange("n p t -> p n t")), writes=[bmT], dma=True)
                    for jb in range(nblk):
                        r = jb - 4 * m
                        Lps, bL = L_rot.next()
                        Pt, bP = P_rot.next()
                        Pm, bPm = Pm_rot.next()
                        near = r >= -1
                        if near:
                            op("tensor", lambda e, Lps=Lps, r=r: e.matmul(Lps[:], lhsT=ident_f, rhs=bt[:, r + 1, :, :].rearrange("p h t -> p (h t)"), start=True, stop=False, skip_group_check=True), reads=[bbt, bconst], writes=[bL])
                        for h in range(4):
                            op("tensor", lambda e, Lps=Lps, h=h, jb=jb, qa_t=qa_t, near=near: e.matmul(Lps[:, h * 128:(h + 1) * 128], lhsT=kT[:, h // 2, jb * 128:(jb + 1) * 128], rhs=qa_t[:, h, :], start=(h == 0 and not near), stop=(h == 3), skip_group_check=True),
                               reads=[bkv, bqa], writes=[bL])
                        op("scalar", lambda e, Lps=Lps, Pt=Pt: e.activation(out=Pt[:], in_=Lps[:], func=AF.Exp), reads=[bL], writes=[bP])
                        op("vector", lambda e, Pt=Pt, Pm=Pm, jb=jb: e.tensor_tensor(out=Pm[:].rearrange("p (h t) -> p h t", h=4), in0=Pt[:].rearrange("p (h t) -> p h t", h=4), in1=maskT[:, jb, :].unsqueeze(1).to_broadcast([128, 4, 128]), op=ALU.mult),
                           reads=[bP, bmT], writes=[bPm])
                        for h in range(4):
                            op("tensor", lambda e, yps=yps, Pm=Pm, h=h, jb=jb, nblk=nblk: e.matmul(yps[:, h * 65:(h + 1) * 65], lhsT=Pm[:, h * 128:(h + 1) * 128], rhs=va_g[:, jb, h, :], start=(jb == 0 and h == 0), stop=(jb == nblk - 1), skip_group_check=True),
                               reads=[bPm, bkv], writes=[byps])
                    op("vector", lambda e, yps=yps: e.tensor_copy(out=den[:], in_=yps[:, 0:260].rearrange("p (h c) -> p h c", h=4)[:, :, 64]), reads=[byps], writes=[bden])
                    op("vector", lambda e: e.reciprocal(out=den[:], in_=den[:]), reads=[bden], writes=[bden])
                    for h in range(4):
                        op("vector", lambda e, yps=yps, h=h, m=m, g=g: e.tensor_scalar(out=ya_sb[:, m, g * 256 + h * 64:g * 256 + (h + 1) * 64], in0=yps[:, h * 65:h * 65 + 64], scalar1=den[:, h:h + 1], scalar2=None, op0=ALU.mult),
                           reads=[byps, bden], writes=[bya])
            R.barrier()
            R.release(mkC)
            if dbg:
                yaf = R.sb([128, NG, 512], F32)
                bt2 = Buf()
                op("vector", lambda e: e.tensor_copy(out=yaf[:], in_=ya_sb[:]), reads=[bya], writes=[bt2])
                op("sync", lambda e: e.dma_start(out=dbg_d["ya"], in_=yaf[:]), reads=[bt2], dma=True)
                R.barrier()
                R.release(mkC)

        if stop_after >= "D":
            mkD = R.mark()
            wpa = R.sb([128, 4, 1024], BF16)
            wpb = R.sb([128, 4, 1024], BF16)
            wout = R.sb([128, 8, 1024], BF16)
            bw = Buf()
            for wt, wd in ((wpa, wpa_d), (wpb, wpb_d), (wout, wout_d)):
                op("gpsimd", lambda e, wt=wt, wd=wd: e.dma_start(out=wt[:], in_=wd.rearrange("(c p) n -> p c n", p=128)), writes=[bw], dma=True)
            yT_rot = Rot([R.sb([128, 8, 128], BF16) for _ in range(2)])
            gt_rot = Rot([R.sb([128, 16, 128], BF16) for _ in range(2)])
            t1_rot = Rot([R.sb([128, 512], F32) for _ in range(2)])
            t2_rot = Rot([R.sb([128, 512], F32) for _ in range(2)])
            mg_rot = Rot([R.sb([128, 8, 128], BF16) for _ in range(2)])
            xo_rot = Rot([R.sb([128, 1024], F32) for _ in range(2)])
            x2_rot = Rot([R.sb([128, 1024], F32) for _ in range(2)])
            tpD = Rot([PS[0]])
            paD = Rot([PS[1], PS[2]])
            pbD = Rot([PS[3], PS[4]])
            oD = Rot([PS[5], PS[6]])
            for m in range(NG):
                yT, byT = yT_rot.next()
                gt, bgt = gt_rot.next()
                mg, bmg = mg_rot.next()
                xo, bxo = xo_rot.next()
                x2t, bx2 = x2_rot.next()
                op("sync", lambda e, gt=gt, m=m: e.dma_start(out=gt[:], in_=gT_s[:, :, m * 128:(m + 1) * 128].rearrange("o p t -> p o t")), writes=[bgt], dma=True)
                op("sync", lambda e, xo=xo, m=m: e.dma_start(out=xo[:], in_=xo_d[m * 128:(m + 1) * 128, :]), writes=[bxo], dma=True)
                tp, btp = tpD.next()
                tpb = tp[:].bitcast(BF16)
                for c in range(4):
                    op("tensor", lambda e, tpb=tpb, c=c, m=m: e.transpose(out=tpb[:, c * 128:(c + 1) * 128], in_=ya_sb[:, m, c * 128:(c + 1) * 128], identity=ident_b[:]), reads=[bya, bconst], writes=[btp])
                    op("tensor", lambda e, tpb=tpb, c=c, m=m: e.transpose(out=tpb[:, (4 + c) * 128:(5 + c) * 128], in_=yb_sb[:, m, c * 128:(c + 1) * 128], identity=ident_b[:]), reads=[byb, bconst], writes=[btp])
                op("scalar", lambda e, tpb=tpb, yT=yT: e.copy(out=yT[:].rearrange("p c t -> p (c t)"), in_=tpb[:, 0:1024]), reads=[btp], writes=[byT])
                for half in range(2):
                    pa, bpa = paD.next()
                    pb, bpb = pbD.next()
                    t1, bt1 = t1_rot.next()
                    t2, bt2_ = t2_rot.next()
                    for bq_ in range(4):
                        blk = half * 4 + bq_
                        for c in range(4):
                            op("tensor", lambda e, pa=pa, bq_=bq_, blk=blk, c=c, yT=yT: e.matmul(pa[:, bq_ * 128:(bq_ + 1) * 128], lhsT=wpa[:, c, blk * 128:(blk + 1) * 128], rhs=yT[:, c, :], start=(c == 0), stop=(c == 3)), reads=[bw, byT], writes=[bpa])
                        for c in range(4):
                            op("tensor", lambda e, pb=pb, bq_=bq_, blk=blk, c=c, yT=yT: e.matmul(pb[:, bq_ * 128:(bq_ + 1) * 128], lhsT=wpb[:, c, blk * 128:(blk + 1) * 128], rhs=yT[:, 4 + c, :], start=(c == 0), stop=(c == 3)), reads=[bw, byT], writes=[bpb])
                    op("vector", lambda e, pa=pa, t1=t1, gt=gt, half=half: e.tensor_tensor(out=t1[:], in0=pa[:], in1=gt[:, half * 4:half * 4 + 4, :].rearrange("p o t -> p (o t)"), op=ALU.mult), reads=[bpa, bgt], writes=[bt1])
                    op("vector", lambda e, pb=pb, t2=t2, gt=gt, half=half: e.tensor_tensor(out=t2[:], in0=pb[:], in1=gt[:, 8 + half * 4:12 + half * 4, :].rearrange("p o t -> p (o t)"), op=ALU.mult), reads=[bpb, bgt], writes=[bt2_])
                    op("gpsimd", lambda e, t1=t1, t2=t2, mg=mg, half=half: e.tensor_tensor(out=mg[:, half * 4:half * 4 + 4, :].rearrange("p o t -> p (o t)"), in0=t1[:], in1=t2[:], op=ALU.add), reads=[bt1, bt2_], writes=[bmg])
                for half in range(2):
                    o2, bo2 = oD.next()
                    for c in range(8):
                        op("tensor", lambda e, o2=o2, c=c, mg=mg, half=half: e.matmul(o2[:], lhsT=mg[:, c, :], rhs=wout[:, c, half * 512:(half + 1) * 512], start=(c == 0), stop=(c == 7)), reads=[bw, bmg], writes=[bo2])
                    hs = slice(half * 512, (half + 1) * 512)
                    op("vector", lambda e, o2=o2, x2t=x2t, hs=hs: e.tensor_tensor(out=x2t[:, hs], in0=o2[:], in1=gate1[:, hs], op=ALU.mult), reads=[bo2, bmod], writes=[bx2])
                    op("gpsimd", lambda e, x2t=x2t, xo=xo, hs=hs: e.tensor_tensor(out=x2t[:, hs], in0=x2t[:, hs], in1=xo[:, hs], op=ALU.add), reads=[bx2, bxo], writes=[bx2])
                op("sync", lambda e, x2t=x2t, m=m: e.dma_start(out=x2_s[m * 128:(m + 1) * 128, :], in_=x2t[:]), reads=[bx2], dma=True)
                if dbg:
                    op("sync", lambda e, x2t=x2t, m=m: e.dma_start(out=dbg_d["x2"][m * 128:(m + 1) * 128, :], in_=x2t[:]), reads=[bx2], dma=True)
            R.barrier()
            R.release(mkD)

        if stop_after >= "E":
            R.release(mkY)
            mkE = R.mark()
            TGT = min(2, NG)
            TG = 128 * TGT
            wqb = R.sb([128, 8, 2048], BF16)
            skb = R.sb([128, 16, 128], BF16)
            bwE = Buf()
            op("gpsimd", lambda e: e.dma_start(out=wqb[:], in_=wq_d.rearrange("(c p) n -> p c n", p=128)), writes=[bwE], dma=True)
            op("gpsimd", lambda e: e.dma_start(out=skb[:], in_=skT_d), writes=[bwE], dma=True)
            x2g = R.sb([128, TGT, 1024], F32)
            bx2g = Buf()
            ntmp = R.sb([128, 1024], F32)
            bnt = Buf()
            hn2 = R.sb([128, TGT, 1024], BF16)
            bhn2 = Buf()
            hn2T = R.sb([128, 8, TG], BF16)
            bhn2T = Buf()
            ssE = R.sb([128, TGT], F32)
            bssE = Buf()
            junk = R.sb([128, 1024], BF16)
            bjk = Buf()
            qT_sb = R.sb([128, 16, TG], BF16)
            bqT = Buf()
            s_sb = R.sb([128, TGT, 16, 128], F32)
            bs = Buf()
            E_sb = s_sb
            bE = bs
            E1s = R.sb([128, TGT, 8, 128], F32)
            bE1s = Buf()
            tmpk = R.sb([128, 256], F32)
            btk = Buf()
            m8e = R.sb([128, 16, 8], F32)
            negmx = R.sb([128, 16], F32)
            bm8e = Buf()
            Etop = R.sb([128, 16, 16], F32)
            bEt = Buf()
            cand = R.sb([128, 8, 256], F32)
            bcand = Buf()
            ctop = R.sb([128, 8, 16], F32)
            bct = Buf()
            Zs = R.sb([128, 8], F32)
            bZ = Buf()
            E1topS = R.sb([128, 8, 16], F32)
            bE1t = Buf()
            theta = R.sb([128, TGT, 8], F32)
            bth = Buf()
            Pp_rot = Rot([R.sb([128, 512], F32) for _ in range(3)])
            Wh_rot = Rot([R.sb([128, 512], BF16) for _ in range(3)])
            G_rot = Rot([R.sb([128, 4 * TG], BF16) for _ in range(2)])
            coef_rot = Rot([R.sb([128, 4 * TG], BF16) for _ in range(2)])
            u_rot = Rot([R.sb([128, 8, 512], BF16) for _ in range(2)])
            v_rot = Rot([R.sb([128, 4, 1024], BF16) for _ in range(2)])
            x3 = R.sb([128, 1024], F32)
            bx3 = Buf()
            PSB = [Buf() for _ in range(8)]
            NWB = TGT
            WTb = list(range(0, NWB))
            PRb = list(range(NWB, 2 * NWB))
            Fb = list(range(2 * NWB, 2 * NWB + 2 * TGT))
            misc = Rot([0, 1, 2, 3][:2 * NWB])

            def cand_top(first):
                for h in range(8):
                    e1 = (Etop if first else E1topS)
                    i1 = (2 * h if first else h)
                    op("vector", lambda e, h=h, e1=e1, i1=i1: e.tensor_tensor(out=cand[:, h, :].rearrange("p (a b) -> p a b", a=16), in0=e1[:, i1, :].unsqueeze(2).to_broadcast([128, 16, 16]), in1=Etop[:, 2 * h + 1, :].unsqueeze(1).to_broadcast([128, 16, 16]), op=ALU.mult),
                       reads=[bEt, bE1t], writes=[bcand])
                for h in range(8):
                    op("vector", lambda e, h=h: e.max(out=ctop[:, h, 0:8], in_=cand[:, h, :]), reads=[bcand], writes=[bct])
                    op("vector", lambda e, h=h: e.match_replace(out=tmpk[:, 0:256], in_to_replace=ctop[:, h, 0:8], in_values=cand[:, h, :], imm_value=-1.0), reads=[bcand, bct], writes=[btk])
                    op("vector", lambda e, h=h: e.max(out=ctop[:, h, 8:16], in_=tmpk[:, 0:256]), reads=[btk], writes=[bct])

            for gi in range(NG // TGT):
                op("sync", lambda e, gi=gi: e.dma_start(out=x2g[:], in_=x2_s[gi * TG:(gi + 1) * TG, :].rearrange("(tt p) d -> p tt d", p=128)), writes=[bx2g], dma=True)
                for tt in range(TGT):
                    op("scalar", lambda e, tt=tt: e.activation(out=junk[:], in_=x2g[:, tt, :], func=AF.Square, accum_out=ssE[:, tt:tt + 1]), reads=[bx2g], writes=[bjk, bssE])
                op("vector", lambda e: e.tensor_scalar(out=ssE[:], in0=ssE[:], scalar1=1.0 / 1024, scalar2=1e-6, op0=ALU.mult, op1=ALU.add), reads=[bssE], writes=[bssE])
                op("scalar", lambda e: e.sqrt(out=ssE[:], in_=ssE[:]), reads=[bssE], writes=[bssE])
                op("vector", lambda e: e.reciprocal(out=ssE[:], in_=ssE[:]), reads=[bssE], writes=[bssE])
                for tt in range(TGT):
                    op("vector", lambda e, tt=tt: e.scalar_tensor_tensor(out=ntmp[:], in0=x2g[:, tt, :], scalar=ssE[:, tt:tt + 1], in1=gmod2[:], op0=ALU.mult, op1=ALU.mult), reads=[bx2g, bssE, bgm], writes=[bnt])
                    op("gpsimd", lambda e, tt=tt: e.tensor_tensor(out=hn2[:, tt, :], in0=ntmp[:], in1=shift2, op=ALU.add), reads=[bnt, bmod], writes=[bhn2])
                    bi = misc.next()[0]
                    tpb = PS[bi][:].bitcast(BF16)
                    for c in range(8):
                        op("tensor", lambda e, tpb=tpb, tt=tt, c=c: e.transpose(out=tpb[:, c * 128:(c + 1) * 128], in_=hn2[:, tt, c * 128:(c + 1) * 128], identity=ident_b[:]), reads=[bhn2, bconst], writes=[PSB[bi]])
                    op("scalar", lambda e, tpb=tpb, tt=tt: e.copy(out=hn2T[:, :, tt * 128:(tt + 1) * 128], in_=tpb[:, 0:1024].rearrange("p (c t) -> p c t", c=8)), reads=[PSB[bi]], writes=[bhn2T])
                for hc in range(16):
                    bi = misc.next()[0]
                    for c in range(8):
                        op("tensor", lambda e, bi=bi, hc=hc, c=c: e.matmul(PS[bi][:, 0:TG], lhsT=wqb[:, c, hc * 128:(hc + 1) * 128], rhs=hn2T[:, c, :], start=(c == 0), stop=(c == 7)), reads=[bwE, bhn2T], writes=[PSB[bi]])
                    if hc % 2:
                        op("scalar", lambda e, bi=bi, hc=hc: e.copy(out=qT_sb[:, hc, :], in_=PS[bi][:, 0:TG]), reads=[PSB[bi]], writes=[bqT])
                    else:
                        op("vector", lambda e, bi=bi, hc=hc: e.tensor_copy(out=qT_sb[:, hc, :], in_=PS[bi][:, 0:TG]), reads=[PSB[bi]], writes=[bqT])
                for tt in range(TGT):
                    for hcg in range(4):
                        bi = misc.next()[0]
                        for hq in range(4):
                            hc = hcg * 4 + hq
                            op("tensor", lambda e, bi=bi, hq=hq, hc=hc, tt=tt: e.matmul(PS[bi][:, hq * 128:(hq + 1) * 128], lhsT=qT_sb[:, hc, tt * 128:(tt + 1) * 128], rhs=skb[:, hc, :], start=True, stop=True), reads=[bqT, bwE], writes=[PSB[bi]])
                        op("scalar", lambda e, bi=bi, hcg=hcg, tt=tt: e.copy(out=s_sb[:, tt, hcg * 4:(hcg + 1) * 4, :].rearrange("p h n -> p (h n)"), in_=PS[bi][:]), reads=[PSB[bi]], writes=[bs])
                for tt in range(TGT):
                    for hc in range(16):
                        op("vector", lambda e, hc=hc, tt=tt: e.max(out=m8e[:, hc, :], in_=s_sb[:, tt, hc, :]), reads=[bs], writes=[bm8e])
                    op("vector", lambda e: e.tensor_scalar(out=negmx[:], in0=m8e[:, :, 0], scalar1=-1.0, scalar2=None, op0=ALU.mult), reads=[bm8e], writes=[bm8e])
                    for hc in range(16):
                        op("scalar", lambda e, hc=hc, tt=tt: e.activation(out=E_sb[:, tt, hc, :], in_=s_sb[:, tt, hc, :], func=AF.Exp, bias=negmx[:, hc:hc + 1]), reads=[bm8e], writes=[bE])
                    for hc in range(16):
                        op("vector", lambda e, hc=hc, tt=tt: e.max(out=Etop[:, hc, 0:8], in_=E_sb[:, tt, hc, :]), reads=[bE], writes=[bEt])
                        op("vector", lambda e, hc=hc, tt=tt: e.match_replace(out=tmpk[:, 0:128], in_to_replace=Etop[:, hc, 0:8], in_values=E_sb[:, tt, hc, :], imm_value=-1.0), reads=[bE, bEt], writes=[btk])
                        op("vector", lambda e, hc=hc: e.max(out=Etop[:, hc, 8:16], in_=tmpk[:, 0:128]), reads=[btk], writes=[bEt])
                    cand_top(True)
                    op("vector", lambda e: e.tensor_reduce(out=Zs[:], in_=ctop[:], axis=AX.X, op=ALU.add), reads=[bct], writes=[bZ])
                    op("vector", lambda e: e.reciprocal(out=Zs[:], in_=Zs[:]), reads=[bZ], writes=[bZ])
                    op("vector", lambda e: e.tensor_tensor(out=E1topS[:], in0=Etop[:].rearrange("p (h c) k -> p h c k", c=2)[:, :, 0, :], in1=Zs[:].unsqueeze(2).to_broadcast([128, 8, 16]), op=ALU.mult), reads=[bEt, bZ], writes=[bE1t])
                    op("vector", lambda e, tt=tt: e.tensor_tensor(out=E1s[:, tt, :, :], in0=E_sb[:, tt, :, :].rearrange("p (h c) n -> p h c n", c=2)[:, :, 0, :], in1=Zs[:].unsqueeze(2).to_broadcast([128, 8, 128]), op=ALU.mult), reads=[bE, bZ], writes=[bE1s])
                    cand_top(False)
                    op("vector", lambda e, tt=tt: e.tensor_copy(out=theta[:, tt, :], in_=ctop[:, :, 15]), reads=[bct], writes=[bth])
                NCH = 32
                for ch in range(NCH):
                    e0 = ch * 512
                    ut, but = u_rot.next()
                    vt, bvt = v_rot.next()
                    Gt, bG = G_rot.next()
                    cf, bcf = coef_rot.next()
                    op("gpsimd", lambda e, ut=ut, e0=e0: e.dma_start(out=ut[:], in_=uT_d[:, e0:e0 + 512].rearrange("(c p) e -> p c e", p=128)), writes=[but], dma=True)
                    op("gpsimd", lambda e, vt=vt, e0=e0: e.dma_start(out=vt[:], in_=v_d[e0:e0 + 512, :].rearrange("(b p) d -> p b d", p=128)), writes=[bvt], dma=True)
                    first = {bi: True for bi in WTb}
                    for tt in range(TGT):
                        for h in range(8):
                            Pp, bPp = Pp_rot.next()
                            Wh, bWh = Wh_rot.next()
                            op("vector", lambda e, Pp=Pp, tt=tt, h=h, ch=ch: e.tensor_tensor(out=Pp[:].rearrange("p (a b) -> p a b", a=4), in0=E1s[:, tt, h, ch * 4:(ch + 1) * 4].unsqueeze(2).to_broadcast([128, 4, 128]), in1=E_sb[:, tt, 2 * h + 1, :].unsqueeze(1).to_broadcast([128, 4, 128]), op=ALU.mult),
                               reads=[bE1s, bE], writes=[bPp])
                            op("gpsimd", lambda e, Pp=Pp, Wh=Wh, tt=tt, h=h: e.tensor_scalar(out=Wh[:], in0=Pp[:], scalar1=theta[:, tt, h:h + 1], scalar2=None, op0=ALU.is_ge),
                               reads=[bPp, bth], writes=[bWh])
                            op("gpsimd", lambda e, Pp=Pp, Wh=Wh: e.tensor_tensor(out=Wh[:], in0=Wh[:], in1=Pp[:], op=ALU.mult),
                               reads=[bPp, bWh], writes=[bWh])
                            for blk in range(4):
                                reg = blk * TGT + tt
                                bi = WTb[reg // 4]
                                col = (reg % 4) * 128
                                st = first[bi]
                                first[bi] = False
                                op("tensor", lambda e, bi=bi, col=col, Wh=Wh, blk=blk, st=st: e.matmul(PS[bi][:, col:col + 128], lhsT=Wh[:, blk * 128:(blk + 1) * 128], rhs=ident_b[:], start=st, stop=False, skip_group_check=True),
                                   reads=[bWh, bconst], writes=[PSB[bi]])
                    for blk in range(4):
                        bi = PRb[(blk * TGT) // 4]
                        col = ((blk * TGT) % 4) * 128
                        for c in range(8):
                            op("tensor", lambda e, bi=bi, col=col, ut=ut, blk=blk, c=c: e.matmul(PS[bi][:, col:col + TG], lhsT=ut[:, c, blk * 128:(blk + 1) * 128], rhs=hn2T[:, c, :], start=(c == 0), stop=(c == 7)),
                               reads=[but, bhn2T], writes=[PSB[bi]])
                    for k, bi in enumerate(PRb):
                        op("scalar", lambda e, bi=bi, k=k, Gt=Gt: e.activation(out=Gt[:, k * 512:(k + 1) * 512], in_=PS[bi][:], func=AF.Gelu), reads=[PSB[bi]], writes=[bG])
                    for k, bi in enumerate(WTb):
                        op("vector", lambda e, bi=bi, k=k, Gt=Gt, cf=cf: e.tensor_tensor(out=cf[:, k * 512:(k + 1) * 512], in0=PS[bi][:], in1=Gt[:, k * 512:(k + 1) * 512], op=ALU.mult), reads=[PSB[bi], bG], writes=[bcf])
                    for blk in range(4):
                        for tt in range(TGT):
                            cc = (blk * TGT + tt) * 128
                            for half in range(2):
                                bi = Fb[tt * 2 + half]
                                op("tensor", lambda e, bi=bi, cf=cf, cc=cc, vt=vt, blk=blk, half=half, ch=ch: e.matmul(PS[bi][:], lhsT=cf[:, cc:cc + 128], rhs=vt[:, blk, half * 512:(half + 1) * 512], start=(ch == 0 and blk == 0), stop=(ch == NCH - 1 and blk == 3)),
                                   reads=[bcf, bvt], writes=[PSB[bi]])
                for tt in range(TGT):
                    for half in range(2):
                        bi = Fb[tt * 2 + half]
                        hs = slice(half * 512, (half + 1) * 512)
                        op("vector", lambda e, bi=bi, hs=hs: e.tensor_tensor(out=x3[:, hs], in0=PS[bi][:], in1=gate2[:, hs], op=ALU.mult), reads=[PSB[bi], bmod], writes=[bx3])
                    op("gpsimd", lambda e, tt=tt: e.tensor_tensor(out=x3[:], in0=x3[:], in1=x2g[:, tt, :], op=ALU.add), reads=[bx3, bx2g], writes=[bx3])
                    op("scalar", lambda e, tt=tt: e.activation(out=junk[:], in_=x3[:], func=AF.Square, accum_out=ssE[:, tt:tt + 1]), reads=[bx3], writes=[bjk, bssE])
                    op("vector", lambda e, tt=tt: e.tensor_scalar(out=ssE[:, tt:tt + 1], in0=ssE[:, tt:tt + 1], scalar1=1.0 / 1024, scalar2=1e-6, op0=ALU.mult, op1=ALU.add), reads=[bssE], writes=[bssE])
                    op("scalar", lambda e, tt=tt: e.sqrt(out=ssE[:, tt:tt + 1], in_=ssE[:, tt:tt + 1]), reads=[bssE], writes=[bssE])
                    op("vector", lambda e, tt=tt: e.reciprocal(out=ssE[:, tt:tt + 1], in_=ssE[:, tt:tt + 1]), reads=[bssE], writes=[bssE])
                    op("vector", lambda e, tt=tt: e.scalar_tensor_tensor(out=ntmp[:], in0=x3[:], scalar=ssE[:, tt:tt + 1], in1=gf_bc[:], op0=ALU.mult, op1=ALU.mult), reads=[bx3, bssE, bgm], writes=[bnt])
                    op("sync", lambda e, gi=gi, tt=tt: e.dma_start(out=out_d[(gi * TGT + tt) * 128:(gi * TGT + tt + 1) * 128, :], in_=ntmp[:]), reads=[bnt], dma=True)
            R.barrier()
            R.release(mkE)

        R.barrier()
        R.emit()
    return nc


def _t5_bucket(n):
    n = np.asarray(n)
    nf = np.maximum(n, 1).astype(np.float32)
    large = 16 + (np.log(nf / np.float32(16)) / np.float32(math.log(128 / 16)) * np.float32(16)).astype(np.int32)
    large = np.minimum(large, 31)
    return np.where(n < 16, n, large)


def make_inputs(inp, NG=16):
    x = np.asarray(inp["x"], np.float32)
    c = np.asarray(inp["c"], np.float32)
    w_in = np.asarray(inp["w_in"], np.float32)[0]
    offs = np.cumsum([0, 512, 512, 512, 512, 64, 8, 512, 512, 512, 1024, 1024])
    seg = {n: w_in[:, offs[i]:offs[i + 1]] for i, n in enumerate(["qa", "ka", "va", "qi", "ki", "wi", "qb", "kb", "vb", "ga", "gb"])}
    wK = np.ascontiguousarray(np.concatenate([seg["ka"], seg["kb"], seg["ki"]], 1))
    wV = np.ascontiguousarray(np.concatenate([seg["va"], seg["vb"]], 1))
    wQ = np.ascontiguousarray(np.concatenate([seg["qa"], seg["qb"], seg["qi"]], 1))
    wG = np.ascontiguousarray(np.concatenate([seg["ga"], seg["gb"]], 1))
    wWi = np.ascontiguousarray(seg["wi"])
    rel_bias = np.asarray(inp["rel_bias"], np.float32)
    sl = np.arange(128)[:, None]
    tl = np.arange(128)[None, :]
    tiles = []
    for k in range(2):
        n = np.maximum(tl + 128 * k - sl, 0)
        tiles.append(rel_bias[_t5_bucket(n)].transpose(0, 2, 1))
    far = rel_bias[np.full((128, 128), 31)].transpose(0, 2, 1)
    a = np.arange(128)
    ones = np.ones((128, 128), np.float32)
    zeros = np.zeros((128, 128), np.float32)
    percore = []
    for j in range(4):
        consts = np.zeros((15, 128, 128), np.float32)
        consts[0] = np.eye(128)
        consts[1] = -1.0 * (a[:, None] >= a[None, :])
        consts[2] = 1.0
        for r in range(4):
            consts[3 + r] = ones if r < j else ((a[:, None] < a[None, :]) if r == j else zeros)
            consts[7 + r] = ones if r < j else ((a[None, :] <= a[:, None]) if r == j else zeros)
            consts[11 + r] = zeros if r < j else (np.where(a[None, :] <= a[:, None], 0.0, NEG) if r == j else np.full((128, 128), NEG))
        bt = []
        for r in range(-1, 4):
            k = j - r
            bt.append(tiles[k] if k in (0, 1) else far)
        bt.append(far)
        percore.append((consts, np.ascontiguousarray(np.stack(bt, 0))))
    sk = np.asarray(inp["peer_sub_keys"], np.float32)[0]
    skT = np.ascontiguousarray(sk.reshape(16, 128, 128).transpose(2, 0, 1))
    uT = np.ascontiguousarray(np.asarray(inp["peer_u"], np.float32)[0].T)
    shared = {
        "w_ada": np.asarray(inp["w_ada"], np.float32)[0], "b_ada": np.asarray(inp["b_ada"], np.float32)[0][None, :],
        "g1": np.asarray(inp["norm1_g"], np.float32)[0][None, :], "g2": np.asarray(inp["norm2_g"], np.float32)[0][None, :],
        "gf": np.asarray(inp["final_g"], np.float32)[None, :],
        "wK": wK, "wV": wV, "wQ": wQ, "wG": wG, "wWi": wWi,
        "wpa": np.asarray(inp["w_proj_a"], np.float32)[0], "wpb": np.asarray(inp["w_proj_b"], np.float32)[0],
        "wout": np.asarray(inp["w_out"], np.float32)[0], "wq": np.asarray(inp["peer_wq"], np.float32)[0],
        "skT": skT, "uT": uT, "pv": np.asarray(inp["peer_v"], np.float32)[0],
    }
    maps = []
    for core in range(8):
        b, j = core // 4, core % 4
        xo = np.ascontiguousarray(x[b][:512 * NG].reshape(NG, 4, 128, 1024)[:, j].reshape(128 * NG, 1024))
        d = dict(shared)
        d["x"] = np.ascontiguousarray(x[b][:512 * NG])
        d["xo"] = xo
        d["cT"] = np.ascontiguousarray(c[b].reshape(8, 128).T)
        d["consts"], d["biasT"] = percore[j]
        maps.append(d)
    return maps


_NC = {}


def kernel(**inputs):
    maps = make_inputs(inputs)
    if "nc" not in _NC:
        _NC["nc"] = build()
    res = run_bass_kernel_spmd(_NC["nc"], maps, core_ids=list(range(8)))
    out = np.zeros((2, 8192, 1024), np.float32)
    ov = out.reshape(2, 16, 4, 128, 1024)
    for core in range(8):
        b, j = core // 4, core % 4
        ov[b, :, j] = np.asarray(res.results[core]["out"], np.float32).reshape(16, 128, 1024)
    return out
```

```python
import math
import os
import numpy as np
import ml_dtypes
import concourse.bass as bass
import concourse.mybir as mybir
from concourse.bass_utils import run_bass_kernel_spmd
from contextlib import ExitStack

F32 = mybir.dt.float32
BF16 = mybir.dt.bfloat16
AF = mybir.ActivationFunctionType
ALU = mybir.AluOpType
AX = mybir.AxisListType
ENGS = ("sync", "scalar", "vector", "gpsimd", "tensor")
NSLOT = 8
SB_BASE = 16512
SB_LIM = 16512 + 208000
NEG = -1.0e30


class Buf:
    __slots__ = ("name", "w", "r")

    def __init__(self, name=""):
        self.name = name
        self.w = None
        self.r = {}


class Rec:
    def __init__(self, nc, stack):
        self.nc = nc
        self.sem = {}
        for e in ENGS:
            self.sem[("c", e)] = stack.enter_context(nc.semaphore("c_" + e))
        for q in ("sync", "scalar", "gpsimd"):
            for s in range(NSLOT):
                self.sem[("d", q, s)] = stack.enter_context(nc.semaphore(f"d_{q}_{s}"))
        self.ops = {e: [] for e in ENGS}
        self.cnt = {e: 0 for e in ENGS}
        self.dcnt = {q: 0 for q in ("sync", "scalar", "gpsimd")}
        self.seen = {e: {} for e in ENGS}
        self.sb_off = SB_BASE
        self.sb_hi = SB_BASE
        self.uid = 0

    def sb(self, shape, dtype, name=None):
        self.uid += 1
        nbytes = int(np.prod(shape[1:])) * (4 if dtype == F32 else 2)
        nbytes = (nbytes + 63) // 64 * 64
        off = self.sb_off
        self.sb_off += nbytes
        self.sb_hi = max(self.sb_hi, self.sb_off)
        assert self.sb_off <= SB_LIM, f"SBUF overflow {self.sb_off}"
        return self.nc.alloc_sbuf_tensor_at(f"{name or 't'}_{self.uid}", list(shape), dtype, offset=off)

    def mark(self):
        return self.sb_off

    def release(self, m):
        self.sb_off = m

    def op(self, eng, fn, reads=(), writes=(), dma=False):
        waits = {}

        def need(ev):
            if ev is None:
                return
            key, val = ev
            if key == ("c", "tensor") and eng == "tensor" and not dma:
                return
            if waits.get(key, 0) < val:
                waits[key] = val

        for b in reads:
            need(b.w)
        for b in writes:
            need(b.w)
            for k, v in b.r.items():
                need((k, v))
        wl = []
        seen = self.seen[eng]
        for key, val in waits.items():
            if seen.get(key, 0) >= val:
                continue
            seen[key] = val
            wl.append((key, val))
        if dma:
            n = self.dcnt[eng]
            self.dcnt[eng] += 1
            slot = n % NSLOT
            val = 16 * (n // NSLOT + 1)
            key = ("d", eng, slot)
            if n >= NSLOT and seen.get(key, 0) < val - 16:
                wl.append((key, val - 16))
                seen[key] = val - 16
            ev = (key, val)
            inc = 16
        else:
            self.cnt[eng] += 1
            ev = (("c", eng), self.cnt[eng])
            inc = 1
        self.ops[eng].append((fn, wl, ev, inc))
        for b in reads:
            if b.r.get(ev[0], 0) < ev[1]:
                b.r[ev[0]] = ev[1]
        for b in writes:
            b.w = ev
            b.r = {}
        return ev

    def barrier(self):
        evs = []
        for e in ENGS:
            if self.cnt[e]:
                evs.append((("c", e), self.cnt[e]))
        for q, n in self.dcnt.items():
            for s in range(NSLOT):
                if n > s:
                    k = (n - 1 - s) // NSLOT + 1
                    evs.append((("d", q, s), 16 * k))
        for e in ENGS:
            wl = []
            for key, val in evs:
                if key == ("c", e):
                    continue
                if self.seen[e].get(key, 0) < val:
                    self.seen[e][key] = val
                    wl.append((key, val))
            if wl:
                self.ops[e].append((None, wl, None, 0))

    def emit(self):
        nc = self.nc
        with nc.Block() as block:
            for e in ENGS:
                ops = self.ops[e]
                if not ops:
                    continue

                def body(eng, ops=ops):
                    for fn, wl, ev, inc in ops:
                        for key, val in wl:
                            eng.wait_ge(self.sem[key], val)
                        if fn is not None:
                            ins = fn(eng)
                            ins.then_inc(self.sem[ev[0]], inc)

                getattr(block, e)(body)
        self.ops = {e: [] for e in ENGS}


class Rot:
    def __init__(self, items):
        self.items = [(t, Buf()) for t in items]
        self.i = 0

    def next(self):
        it = self.items[self.i % len(self.items)]
        self.i += 1
        return it


def build(stop_after="E", dbg=False, NG=16):
    nc = bass.Bass("TRN2", target_bir_lowering=False)
    S = 512 * NG
    NQ = 128 * NG
    NT = 4 * NG

    def din(name, shape, dt=F32):
        return nc.dram_tensor(name, list(shape), dt, kind="ExternalInput").ap()

    def dscr(name, shape, dt):
        return nc.dram_tensor(name, list(shape), dt).ap()

    x_d = din("x", [S, 1024])
    xo_d = din("xo", [NQ, 1024])
    cT_d = din("cT", [128, 8])
    wada_d = din("w_ada", [1024, 6144])
    bada_d = din("b_ada", [1, 6144])
    g1_d = din("g1", [1, 1024])
    g2_d = din("g2", [1, 1024])
    gf_d = din("gf", [1, 1024])
    wK_d = din("wK", [1024, 1088])
    wV_d = din("wV", [1024, 1024])
    wQ_d = din("wQ", [1024, 1536])
    wG_d = din("wG", [1024, 2048])
    wWi_d = din("wWi", [1024, 8])
    bias_d = din("biasT", [6, 128, 8, 128])
    const_d = din("consts", [15, 128, 128])
    wpa_d = din("wpa", [512, 1024])
    wpb_d = din("wpb", [512, 1024])
    wout_d = din("wout", [1024, 1024])
    wq_d = din("wq", [1024, 2048])
    skT_d = din("skT", [128, 16, 128])
    uT_d = din("uT", [1024, 16384])
    v_d = din("pv", [16384, 1024])
    out_d = nc.dram_tensor("out", [NQ, 1024], F32, kind="ExternalOutput").ap()
    dbg_d = {}
    if dbg:
        dbg_d["yb"] = nc.dram_tensor("dbg_yb", [128, NG, 512], F32, kind="ExternalOutput").ap()
        dbg_d["ya"] = nc.dram_tensor("dbg_ya", [128, NG, 512], F32, kind="ExternalOutput").ap()
        dbg_d["mod"] = nc.dram_tensor("dbg_mod", [128, 6144], F32, kind="ExternalOutput").ap()
        dbg_d["x2"] = nc.dram_tensor("dbg_x2", [NQ, 1024], F32, kind="ExternalOutput").ap()

    kaT_s = dscr("kaT_s", [4, 128, S], BF16)
    kbT_s = dscr("kbT_s", [4, 128, S], BF16)
    kiT_s = dscr("kiT_s", [64, S], BF16)
    va_s = dscr("va_s", [S, 512], BF16)
    vb_s = dscr("vb_s", [S, 512], BF16)
    gT_s = dscr("gT_s", [16, 128, NQ], BF16)
    q_s = dscr("q_s", [24, 128, NQ], BF16)
    maskT_s = dscr("maskT_s", [NG, NT, 128, 128], BF16)
    x2_s = dscr("x2_s", [NQ, 1024], F32)

    with ExitStack() as st:
        R = Rec(nc, st)
        op = R.op
        PS = [nc.alloc_psum_tensor(f"psb{i}", [128, 512], F32) for i in range(8)]

        consts = R.sb([128, 15, 128], F32, "consts")
        bconst = Buf()
        ident_f = consts[:, 0, :]
        tri_f = consts[:, 1, :]
        ones_f = consts[:, 2, :]
        msb_f = consts[:, 3:7, :]
        caus01 = consts[:, 7:11, :]
        causneg = consts[:, 11:15, :]
        ident_b = R.sb([128, 128], BF16, "identb")
        msb_b = R.sb([128, 4, 128], BF16, "msbb")
        mod_bc = R.sb([128, 6144], F32, "mod")
        bmod = Buf()
        gmod1 = R.sb([128, 1024], F32, "gmod1")
        gmod2 = R.sb([128, 1024], F32, "gmod2")
        gf_bc = R.sb([128, 1024], F32, "gf")
        bgm = Buf()
        wi_sb = R.sb([128, NG, 8], F32, "wi")
        bwi = Buf()
        shift1 = mod_bc[:, 0:1024]
        gate1 = mod_bc[:, 2048:3072]
        shift2 = mod_bc[:, 3072:4096]
        gate2 = mod_bc[:, 5120:6144]

        op("sync", lambda e: e.dma_start(out=consts[:], in_=const_d.rearrange("k p n -> p k n")), writes=[bconst], dma=True)
        op("vector", lambda e: e.tensor_copy(out=ident_b[:], in_=ident_f), reads=[bconst], writes=[bconst])
        op("vector", lambda e: e.tensor_copy(out=msb_b[:], in_=msb_f), reads=[bconst], writes=[bconst])

        mk0 = R.mark()
        cT = R.sb([128, 8], F32)
        bc = Buf()
        condrep = R.sb([128, 8, 128], F32)
        bada_bc = R.sb([128, 6144], F32)
        bb = Buf()
        g_bc = R.sb([128, 2, 1024], F32)
        bg = Buf()
        wa_rot = Rot([R.sb([128, 8, 512], F32) for _ in range(2)])
        op("sync", lambda e: e.dma_start(out=cT[:], in_=cT_d), writes=[bc], dma=True)
        op("sync", lambda e: e.dma_start(out=bada_bc[:], in_=bada_d.partition_broadcast(128)), writes=[bb], dma=True)
        op("sync", lambda e: e.dma_start(out=g_bc[:, 0, :], in_=g1_d.partition_broadcast(128)), writes=[bg], dma=True)
        op("sync", lambda e: e.dma_start(out=g_bc[:, 1, :], in_=g2_d.partition_broadcast(128)), writes=[bg], dma=True)
        op("sync", lambda e: e.dma_start(out=gf_bc[:], in_=gf_d.partition_broadcast(128)), writes=[bgm], dma=True)
        op("scalar", lambda e: e.activation(out=cT[:], in_=cT[:], func=AF.Silu), reads=[bc], writes=[bc])
        for c in range(8):
            op("vector", lambda e, c=c: e.tensor_scalar(out=condrep[:, c, :], in0=ones_f, scalar1=cT[:, c:c + 1], scalar2=None, op0=ALU.mult),
               reads=[bc, bconst], writes=[bc])
        ps0_rot = Rot([PS[0], PS[1]])
        for nn in range(12):
            wt, bw = wa_rot.next()
            op("sync", lambda e, wt=wt, nn=nn: e.dma_start(out=wt[:], in_=wada_d[:, nn * 512:(nn + 1) * 512].rearrange("(c p) n -> p c n", p=128)),
               writes=[bw], dma=True)
            ps, bps = ps0_rot.next()
            for c in range(8):
                op("tensor", lambda e, ps=ps, wt=wt, c=c: e.matmul(ps[:], lhsT=condrep[:, c, :], rhs=wt[:, c, :], start=(c == 0), stop=(c == 7)),
                   reads=[bc, bw], writes=[bps])
            op("vector", lambda e, ps=ps, nn=nn: e.tensor_tensor(out=mod_bc[:, nn * 512:(nn + 1) * 512], in0=ps[:], in1=bada_bc[:, nn * 512:(nn + 1) * 512], op=ALU.add),
               reads=[bps, bb], writes=[bmod])
        op("vector", lambda e: e.scalar_tensor_tensor(out=gmod1[:], in0=mod_bc[:, 1024:2048], scalar=1.0, in1=g_bc[:, 0, :], op0=ALU.add, op1=ALU.mult),
           reads=[bmod, bg], writes=[bgm])
        op("vector", lambda e: e.scalar_tensor_tensor(out=gmod2[:], in0=mod_bc[:, 4096:5120], scalar=1.0, in1=g_bc[:, 1, :], op0=ALU.add, op1=ALU.mult),
           reads=[bmod, bg], writes=[bgm])
        if dbg:
            op("sync", lambda e: e.dma_start(out=dbg_d["mod"], in_=mod_bc[:]), reads=[bmod], dma=True)
        R.barrier()
        R.release(mk0)

        mkA = R.mark()
        wK = R.sb([128, 8, 1088], BF16)
        wV = R.sb([128, 8, 1024], BF16)
        bw = Buf()
        for wt, wd in ((wK, wK_d), (wV, wV_d)):
            op("gpsimd", lambda e, wt=wt, wd=wd: e.dma_start(out=wt[:], in_=wd.rearrange("(c p) n -> p c n", p=128)), writes=[bw], dma=True)
        kst_rot = Rot([R.sb([128, 9, 512], BF16) for _ in range(1)])
        vst_rot = Rot([R.sb([128, 4, 1024], BF16) for _ in range(1)])
        psT_rot = Rot([PS[0], PS[1]])
        psM_rot = Rot([PS[2], PS[3], PS[4], PS[5], PS[6], PS[7]])
        W = {}

        def alloc_norm_work():
            W["xt"] = Rot([R.sb([128, 4, 1024], F32) for _ in range(1)])
            W["hn"] = Rot([R.sb([128, 4, 1024], BF16) for _ in range(1)])
            W["hnT"] = Rot([R.sb([128, 8, 512], BF16) for _ in range(2)])
            W["junk"] = R.sb([128, 1024], BF16)
            W["ss"] = Rot([R.sb([128, 4], F32) for _ in range(2)])

        alloc_norm_work()
        bjunk = Buf()
        evac_i = [0]

        def evac(out_ap, in_ap, reads, writes, func=None, scale=None):
            if func is not None:
                op("scalar", lambda e: e.activation(out=out_ap, in_=in_ap, func=func), reads=reads, writes=writes)
                return
            evac_i[0] += 1
            if scale is not None:
                if evac_i[0] % 2:
                    op("scalar", lambda e: e.mul(out=out_ap, in_=in_ap, mul=scale), reads=reads, writes=writes)
                else:
                    op("vector", lambda e: e.tensor_scalar(out=out_ap, in0=in_ap, scalar1=scale, scalar2=None, op0=ALU.mult), reads=reads, writes=writes)
                return
            if evac_i[0] % 2:
                op("scalar", lambda e: e.copy(out=out_ap, in_=in_ap), reads=reads, writes=writes)
            else:
                op("vector", lambda e: e.tensor_copy(out=out_ap, in_=in_ap), reads=reads, writes=writes)

        def norm_group(src_ap, nt):
            xt, bx = W["xt"].next()
            hn, bhn = W["hn"].next()
            hnT, bhnT = W["hnT"].next()
            ss, bss = W["ss"].next()
            sq_junk = W["junk"]
            op("sync", lambda e: e.dma_start(out=xt[:, 0:nt, :], in_=src_ap.rearrange("(tt p) d -> p tt d", p=128)), writes=[bx], dma=True)
            for tt in range(nt):
                op("scalar", lambda e, tt=tt: e.activation(out=sq_junk[:], in_=xt[:, tt, :], func=AF.Square, accum_out=ss[:, tt:tt + 1]),
                   reads=[bx], writes=[bjunk, bss])
            op("vector", lambda e: e.tensor_scalar(out=ss[:, 0:nt], in0=ss[:, 0:nt], scalar1=1.0 / 1024, scalar2=1e-6, op0=ALU.mult, op1=ALU.add), reads=[bss], writes=[bss])
            op("scalar", lambda e: e.sqrt(out=ss[:, 0:nt], in_=ss[:, 0:nt]), reads=[bss], writes=[bss])
            op("vector", lambda e: e.reciprocal(out=ss[:, 0:nt], in_=ss[:, 0:nt]), reads=[bss], writes=[bss])
            for tt in range(nt):
                op("vector", lambda e, tt=tt: e.scalar_tensor_tensor(out=xt[:, tt, :], in0=xt[:, tt, :], scalar=ss[:, tt:tt + 1], in1=gmod1[:], op0=ALU.mult, op1=ALU.mult),
                   reads=[bx, bss, bgm], writes=[bx])
                op("gpsimd", lambda e, tt=tt: e.tensor_tensor(out=hn[:, tt, :], in0=xt[:, tt, :], in1=shift1, op=ALU.add),
                   reads=[bx, bmod], writes=[bhn])
            for tt in range(nt):
                pst, bpst = psT_rot.next()
                pstb = pst[:].bitcast(BF16)
                for c in range(8):
                    op("tensor", lambda e, pstb=pstb, tt=tt, c=c: e.transpose(out=pstb[:, c * 128:(c + 1) * 128], in_=hn[:, tt, c * 128:(c + 1) * 128], identity=ident_b[:]),
                       reads=[bhn, bconst], writes=[bpst])
                evac(hnT[:, :, tt * 128:(tt + 1) * 128], pstb.rearrange("p (c t) -> p c t", c=8), [bpst], [bhnT])
            return hnT, bhnT

        for m in range(NG):
            hnT, bhnT = norm_group(x_d[m * 512:(m + 1) * 512, :], 4)
            kst, bkst = kst_rot.next()
            vst, bvst = vst_rot.next()
            for o in range(9):
                wdt = 128 if o < 8 else 64
                ps, bps = psM_rot.next()
                for c in range(8):
                    op("tensor", lambda e, ps=ps, hnT=hnT, o=o, c=c, wdt=wdt: e.matmul(ps[0:wdt, :], lhsT=wK[:, c, o * 128:o * 128 + wdt], rhs=hnT[:, c, :], start=(c == 0), stop=(c == 7)),
                       reads=[bw, bhnT], writes=[bps])
                evac(kst[0:wdt, o, :], ps[0:wdt, :], [bps], [bkst])
            op("sync", lambda e, kst=kst, m=m: e.dma_start(out=kaT_s[:, :, m * 512:(m + 1) * 512].rearrange("o p t -> p o t"), in_=kst[:, 0:4, :]), reads=[bkst], dma=True)
            op("sync", lambda e, kst=kst, m=m: e.dma_start(out=kbT_s[:, :, m * 512:(m + 1) * 512].rearrange("o p t -> p o t"), in_=kst[:, 4:8, :]), reads=[bkst], dma=True)
            op("sync", lambda e, kst=kst, m=m: e.dma_start(out=kiT_s[:, m * 512:(m + 1) * 512], in_=kst[0:64, 8, :]), reads=[bkst], dma=True)
            for tt in range(4):
                for half in range(2):
                    ps, bps = psM_rot.next()
                    for c in range(8):
                        op("tensor", lambda e, ps=ps, hnT=hnT, tt=tt, half=half, c=c: e.matmul(ps[:], lhsT=hnT[:, c, tt * 128:(tt + 1) * 128], rhs=wV[:, c, half * 512:(half + 1) * 512], start=(c == 0), stop=(c == 7)),
                           reads=[bw, bhnT], writes=[bps])
                    evac(vst[:, tt, half * 512:(half + 1) * 512], ps[:], [bps], [bvst])
            op("sync", lambda e, vst=vst, m=m: e.dma_start(out=va_s[m * 512:(m + 1) * 512, :].rearrange("(tt p) c -> p tt c", p=128), in_=vst[:, :, 0:512]), reads=[bvst], dma=True)
            op("sync", lambda e, vst=vst, m=m: e.dma_start(out=vb_s[m * 512:(m + 1) * 512, :].rearrange("(tt p) c -> p tt c", p=128), in_=vst[:, :, 512:1024]), reads=[bvst], dma=True)

        R.barrier()
        R.release(mkA)
        wQ = R.sb([128, 8, 1536], BF16)
        wG = R.sb([128, 8, 2048], BF16)
        wWi = R.sb([128, 8, 8], BF16)
        bw = Buf()
        for wt, wd in ((wQ, wQ_d), (wG, wG_d), (wWi, wWi_d)):
            op("gpsimd", lambda e, wt=wt, wd=wd: e.dma_start(out=wt[:], in_=wd.rearrange("(c p) n -> p c n", p=128)), writes=[bw], dma=True)
        gst_rot = Rot([R.sb([128, 512], BF16) for _ in range(3)])
        zt = R.sb([128, 512], BF16)
        bzt = Buf()
        op("vector", lambda e: e.memset(zt[:], 0.0), writes=[bzt])
        alloc_norm_work()
        for mm in range((NG + 3) // 4):
            nt = min(4, NG - 4 * mm)
            NW = nt * 128
            hnT, bhnT = norm_group(xo_d[mm * 512:mm * 512 + NW, :], nt)
            for og in range(3):
                for oo in range(4):
                    o = og * 4 + oo
                    ps, bps = psM_rot.next()
                    for c in range(8):
                        op("tensor", lambda e, ps=ps, hnT=hnT, o=o, c=c, NW=NW: e.matmul(ps[:, 0:NW], lhsT=wQ[:, c, o * 128:(o + 1) * 128], rhs=hnT[:, c, 0:NW], start=(c == 0), stop=(c == 7)),
                           reads=[bw, bhnT], writes=[bps])
                    gst, bgst = gst_rot.next()
                    evac(gst[:, 0:NW], ps[:, 0:NW], [bps], [bgst], scale=(0.125 if og < 2 else None))
                    cs = slice(mm * 512, mm * 512 + NW)
                    op("sync", lambda e, gst=gst, o=o, cs=cs, NW=NW: e.dma_start(out=q_s[2 * o, 0:64, cs], in_=gst[0:64, 0:NW]), reads=[bgst], dma=True)
                    op("sync", lambda e, o=o, cs=cs, NW=NW: e.dma_start(out=q_s[2 * o, 64:128, cs], in_=zt[64:128, 0:NW]), reads=[bzt], dma=True)
                    op("sync", lambda e, gst=gst, o=o, cs=cs, NW=NW: e.dma_start(out=q_s[2 * o + 1, 64:128, cs], in_=gst[64:128, 0:NW]), reads=[bgst], dma=True)
                    op("sync", lambda e, o=o, cs=cs, NW=NW: e.dma_start(out=q_s[2 * o + 1, 0:64, cs], in_=zt[0:64, 0:NW]), reads=[bzt], dma=True)
            for o in range(16):
                ps, bps = psM_rot.next()
                gst, bgst = gst_rot.next()
                for c in range(8):
                    op("tensor", lambda e, ps=ps, hnT=hnT, o=o, c=c, NW=NW: e.matmul(ps[:, 0:NW], lhsT=wG[:, c, o * 128:(o + 1) * 128], rhs=hnT[:, c, 0:NW], start=(c == 0), stop=(c == 7)),
                       reads=[bw, bhnT], writes=[bps])
                evac(gst[:, 0:NW], ps[:, 0:NW], [bps], [bgst], func=AF.Sigmoid)
                op("sync", lambda e, gst=gst, o=o, mm=mm, NW=NW: e.dma_start(out=gT_s[o, :, mm * 512:mm * 512 + NW], in_=gst[:, 0:NW]), reads=[bgst], dma=True)
            for tt in range(nt):
                ps, bps = psM_rot.next()
                for c in range(8):
                    op("tensor", lambda e, ps=ps, hnT=hnT, c=c, tt=tt: e.matmul(ps[:, 0:8], lhsT=hnT[:, c, tt * 128:(tt + 1) * 128], rhs=wWi[:, c, :], start=(c == 0), stop=(c == 7)),
                       reads=[bw, bhnT], writes=[bps])
                op("vector", lambda e, ps=ps, mm=mm, tt=tt: e.tensor_copy(out=wi_sb[:, mm * 4 + tt, :], in_=ps[:, 0:8]), reads=[bps], writes=[bwi])
        R.barrier()
        R.release(mkA)
        mkY = R.mark()
        yb_sb = R.sb([128, NG, 512], BF16, "yb")
        ya_sb = R.sb([128, NG, 512], BF16, "ya")
        bya = Buf()
        byb = Buf()

        if stop_after >= "B":
            mkB = R.mark()
            kT = R.sb([128, 2, S], BF16)
            vg = R.sb([128, NT, 256], BF16)
            bkv = Buf()
            qbT = R.sb([128, 4, NQ], BF16)
            bq = Buf()
            e_rot = Rot([R.sb([128, 512], F32) for _ in range(2)])
            sp_rot = Rot([R.sb([128, 512], F32) for _ in range(3)])
            arg_rot = Rot([R.sb([128, 512], F32) for _ in range(2)])
            A_rot = Rot([R.sb([128, 512], BF16) for _ in range(3)])
            carry_rot = Rot([R.sb([128, 512], F32) for _ in range(2)])
            z_rot = Rot([PS[0], PS[1], PS[2]])
            c_rot = Rot([PS[3], PS[4], PS[5]])
            y_rot = Rot([PS[6], PS[7]])
            for g in range(2):
                op("sync", lambda e, g=g: e.dma_start(out=qbT[:], in_=q_s[8 + 4 * g:12 + 4 * g].rearrange("o p t -> p o t")), writes=[bq], dma=True)
                op("sync", lambda e, g=g: e.dma_start(out=kT[:], in_=kbT_s[2 * g:2 * g + 2].rearrange("o p t -> p o t")), writes=[bkv], dma=True)
                for n8 in range(0, NT, 8):
                    ne = min(NT, n8 + 8)
                    op("sync", lambda e, g=g, n8=n8, ne=ne: e.dma_start(out=vg[:, n8:ne, :], in_=vb_s[n8 * 128:ne * 128, g * 256:(g + 1) * 256].rearrange("(n p) c -> p n c", p=128)), writes=[bkv], dma=True)
                for m in range(NG):
                    jtop = 4 * m + 3
                    carry, bcar = carry_rot.next()
                    yps, byps = y_rot.next()
                    op("gpsimd", lambda e, carry=carry: e.memset(carry[:], 0.0), writes=[bcar])
                    steps = list(range(jtop, -1, -1))
                    ctx = {}

                    def stage1(k, m=m, g=g):
                        jb = steps[k]
                        r = jb - 4 * m
                        zps, bz = z_rot.next()
                        et, be = e_rot.next()
                        sp, bsp = sp_rot.next()
                        ctx[k] = (zps, bz, sp, bsp)
                        for h in range(4):
                            op("tensor", lambda e, zps=zps, h=h, jb=jb: e.matmul(zps[:, h * 128:(h + 1) * 128], lhsT=kT[:, h // 2, jb * 128:(jb + 1) * 128], rhs=qbT[:, h, m * 128:(m + 1) * 128], start=(h == 0), stop=False, skip_group_check=True),
                               reads=[bkv, bq], writes=[bz])
                        op("scalar", lambda e, et=et, zps=zps: e.activation(out=et[:], in_=zps[:], func=AF.Exp), reads=[bz], writes=[be])
                        op("scalar", lambda e, et=et, sp=sp: e.activation(out=sp[:], in_=et[:], func=AF.Ln, bias=1.0), reads=[be], writes=[bsp])
                        if r >= 0:
                            op("vector", lambda e, sp=sp, r=r: e.tensor_tensor(out=sp[:].rearrange("p (h t) -> p h t", h=4), in0=sp[:].rearrange("p (h t) -> p h t", h=4), in1=msb_f[:, r, :].unsqueeze(1).to_broadcast([128, 4, 128]), op=ALU.mult),
                               reads=[bsp, bconst], writes=[bsp])

                    def stage2(k, m=m, carry=carry, bcar=bcar):
                        jb = steps[k]
                        r = jb - 4 * m
                        zps, bz, sp, bsp = ctx[k]
                        cps, bcp = c_rot.next()
                        arg, barg = arg_rot.next()
                        At, bA = A_rot.next()
                        ctx[k] = (At, bA)
                        op("tensor", lambda e, zps=zps, sp=sp: e.matmul(zps[:], lhsT=tri_f, rhs=sp[:], start=False, stop=True, skip_group_check=True), reads=[bsp, bconst], writes=[bz])
                        op("tensor", lambda e, cps=cps, sp=sp: e.matmul(cps[:], lhsT=ones_f, rhs=sp[:], start=True, stop=True), reads=[bsp, bconst], writes=[bcp])
                        op("vector", lambda e, arg=arg, zps=zps: e.tensor_tensor(out=arg[:], in0=zps[:], in1=carry[:], op=ALU.subtract), reads=[bz, bcar], writes=[barg])
                        op("vector", lambda e, cps=cps: e.tensor_tensor(out=carry[:], in0=cps[:], in1=carry[:], op=ALU.add), reads=[bcp, bcar], writes=[bcar])
                        op("scalar", lambda e, At=At, arg=arg: e.activation(out=At[:], in_=arg[:], func=AF.Exp), reads=[barg], writes=[bA])
                        if r >= 0:
                            op("gpsimd", lambda e, At=At, r=r: e.tensor_tensor(out=At[:].rearrange("p (h t) -> p h t", h=4), in0=At[:].rearrange("p (h t) -> p h t", h=4), in1=msb_b[:, r, :].unsqueeze(1).to_broadcast([128, 4, 128]), op=ALU.mult),
                               reads=[bA, bconst], writes=[bA])

                    def stage3(k, yps=yps, byps=byps, jtop=jtop):
                        jb = steps[k]
                        At, bA = ctx.pop(k)
                        for h in range(4):
                            op("tensor", lambda e, At=At, h=h, jb=jb: e.matmul(yps[:, h * 64:(h + 1) * 64], lhsT=At[:, h * 128:(h + 1) * 128], rhs=vg[:, jb, h * 64:(h + 1) * 64], start=(jb == jtop and h == 0), stop=(jb == 0), skip_group_check=True),
                               reads=[bA, bkv], writes=[byps])

                    nst = len(steps)
                    for i in range(nst + 2):
                        if i < nst:
                            stage1(i)
                        if 1 <= i <= nst:
                            stage2(i - 1)
                        if i >= 2:
                            stage3(i - 2)
                    op("vector", lambda e, yps=yps, m=m, g=g: e.tensor_copy(out=yb_sb[:, m, g * 256:(g + 1) * 256], in_=yps[:, 0:256]), reads=[byps], writes=[byb])
            R.barrier()
            R.release(mkB)
            if dbg:
                ybf = R.sb([128, NG, 512], F32)
                bt = Buf()
                op("vector", lambda e: e.tensor_copy(out=ybf[:], in_=yb_sb[:]), reads=[byb], writes=[bt])
                op("sync", lambda e: e.dma_start(out=dbg_d["yb"], in_=ybf[:]), reads=[bt], dma=True)
                R.barrier()
                R.release(mkB)

        if stop_after >= "C":
            mkC = R.mark()
            kiT2 = R.sb([128, S], BF16)
            bki = Buf()
            op("sync", lambda e: e.dma_start(out=kiT2[0:64, :], in_=kiT_s), writes=[bki], dma=True)
            op("sync", lambda e: e.dma_start(out=kiT2[64:128, :], in_=kiT_s), writes=[bki], dma=True)
            score = R.sb([128, S], F32)
            bsc = Buf()
            work = R.sb([128, S], F32)
            bwk = Buf()
            mask01 = R.sb([128, S], BF16)
            bmk = Buf()
            qi_rot = Rot([R.sb([128, 8, 128], BF16) for _ in range(2)])
            r_rot = Rot([R.sb([128, 512], F32) for _ in range(3)])
            m8_rot = Rot([R.sb([128, 8], F32) for _ in range(2)])
            thr = R.sb([128, 1], F32)
            bthr = Buf()
            NBIS = 24
            blo = R.sb([128, 1], F32)
            bhi = R.sb([128, 1], F32)
            bmid = R.sb([128, 1], F32)
            bcnt = R.sb([128, 1], F32)
            bge = R.sb([128, 1], mybir.dt.uint32)
            blt = R.sb([128, 1], mybir.dt.uint32)
            half_c = R.sb([128, 1], F32)
            bbis = Buf()
            op("vector", lambda e: e.memset(half_c[:], 0.5), writes=[bbis])
            mst_rot = Rot([R.sb([128, 8, 128], BF16) for _ in range(2)])
            sc_rot = Rot([PS[0], PS[1], PS[2], PS[3]])
            tp_rot = Rot([PS[4], PS[5]])
            for m in range(NG):
                nblk = 4 * m + 4
                nk = nblk * 128
                qi_t, bqi = qi_rot.next()
                op("sync", lambda e, qi_t=qi_t, m=m: e.dma_start(out=qi_t[:], in_=q_s[16:24, :, m * 128:(m + 1) * 128].rearrange("o p t -> p o t")), writes=[bqi], dma=True)
                for ck in range(m + 1):
                    cs = slice(ck * 512, (ck + 1) * 512)
                    for h in range(8):
                        ps, bps = sc_rot.next()
                        rt, brt = r_rot.next()
                        op("tensor", lambda e, ps=ps, qi_t=qi_t, h=h, cs=cs: e.matmul(ps[:], lhsT=qi_t[:, h, :], rhs=kiT2[:, cs], start=True, stop=True), reads=[bqi, bki], writes=[bps])
                        op("scalar", lambda e, ps=ps, rt=rt: e.activation(out=rt[:], in_=ps[:], func=AF.Relu), reads=[bps], writes=[brt])
                        if h == 0:
                            op("vector", lambda e, rt=rt, cs=cs, m=m: e.tensor_scalar(out=score[:, cs], in0=rt[:], scalar1=wi_sb[:, m, 0:1], scalar2=None, op0=ALU.mult), reads=[brt, bwi], writes=[bsc])
                        else:
                            op("vector", lambda e, rt=rt, cs=cs, m=m, h=h: e.scalar_tensor_tensor(out=score[:, cs], in0=rt[:], scalar=wi_sb[:, m, h:h + 1], in1=score[:, cs], op0=ALU.mult, op1=ALU.add), reads=[brt, bwi, bsc], writes=[bsc])
                last = slice(4 * m * 128, nk)
                op("vector", lambda e, last=last: e.tensor_tensor(out=score[:, last].rearrange("p (r s) -> p r s", r=4), in0=score[:, last].rearrange("p (r s) -> p r s", r=4), in1=caus01, op=ALU.mult), reads=[bsc, bconst], writes=[bsc])
                op("vector", lambda e, nk=nk: e.tensor_reduce(out=blo[:], in_=score[:, 0:nk], axis=AX.X, op=ALU.min), reads=[bsc], writes=[bbis])
                op("vector", lambda e: e.tensor_scalar(out=blo[:], in0=blo[:], scalar1=-1.0, scalar2=None, op0=ALU.add), reads=[bbis], writes=[bbis])
                op("vector", lambda e, last=last: e.tensor_tensor(out=score[:, last].rearrange("p (r s) -> p r s", r=4), in0=score[:, last].rearrange("p (r s) -> p r s", r=4), in1=causneg, op=ALU.add), reads=[bsc, bconst], writes=[bsc])
                op("vector", lambda e, nk=nk: e.tensor_reduce(out=bhi[:], in_=score[:, 0:nk], axis=AX.X, op=ALU.max), reads=[bsc], writes=[bbis])
                op("vector", lambda e: e.tensor_scalar(out=bhi[:], in0=bhi[:], scalar1=1.0, scalar2=None, op0=ALU.add), reads=[bbis], writes=[bbis])
                for it in range(NBIS):
                    op("vector", lambda e: e.scalar_tensor_tensor(out=bmid[:], in0=blo[:], scalar=bhi[:, 0:1], in1=half_c[:], op0=ALU.add, op1=ALU.mult), reads=[bbis], writes=[bbis])
                    op("vector", lambda e, nk=nk: e.tensor_scalar(out=work[:, 0:nk], in0=score[:, 0:nk], scalar1=bmid[:, 0:1], scalar2=0.0, op0=ALU.is_ge, op1=ALU.add, accum_out=bcnt[:]), reads=[bsc, bbis], writes=[bwk, bbis])
                    op("vector", lambda e: e.tensor_scalar(out=bge[:], in0=bcnt[:], scalar1=256.0, scalar2=None, op0=ALU.is_ge), reads=[bbis], writes=[bbis])
                    op("vector", lambda e: e.tensor_scalar(out=blt[:], in0=bcnt[:], scalar1=256.0, scalar2=None, op0=ALU.is_lt), reads=[bbis], writes=[bbis])
                    op("vector", lambda e: e.copy_predicated(out=blo[:], mask=bge[:], data=bmid[:]), reads=[bbis], writes=[bbis])
                    op("vector", lambda e: e.copy_predicated(out=bhi[:], mask=blt[:], data=bmid[:]), reads=[bbis], writes=[bbis])
                op("vector", lambda e: e.tensor_copy(out=thr[:], in_=blo[:]), reads=[bbis], writes=[bthr])
                op("vector", lambda e, nk=nk: e.tensor_scalar(out=mask01[:, 0:nk], in0=score[:, 0:nk], scalar1=thr[:, 0:1], scalar2=None, op0=ALU.is_ge), reads=[bsc, bthr], writes=[bmk])
                for b0 in range(0, nblk, 8):
                    nb = min(8, nblk - b0)
                    tp, btp = tp_rot.next()
                    tpb = tp[:].bitcast(BF16)
                    mst, bmst = mst_rot.next()
                    for bb_ in range(nb):
                        op("tensor", lambda e, tpb=tpb, bb_=bb_, b0=b0: e.transpose(out=tpb[:, bb_ * 128:(bb_ + 1) * 128], in_=mask01[:, (b0 + bb_) * 128:(b0 + bb_ + 1) * 128], identity=ident_b[:]), reads=[bmk, bconst], writes=[btp])
                    op("scalar", lambda e, tpb=tpb, mst=mst, nb=nb: e.copy(out=mst[:, 0:nb, :], in_=tpb[:, 0:nb * 128].rearrange("p (n t) -> p n t", n=nb)), reads=[btp], writes=[bmst])
                    op("sync", lambda e, mst=mst, m=m, b0=b0, nb=nb: e.dma_start(out=maskT_s[m, b0:b0 + nb].rearrange("n p t -> p n t"), in_=mst[:, 0:nb, :]), reads=[bmst], dma=True)
            R.barrier()
            R.release(mkC)

            kT = R.sb([128, 2, S], BF16)
            va_g = R.sb([128, NT, 4, 65], BF16)
            bkv = Buf()
            vtmp_rot = Rot([R.sb([128, 8, 256], BF16) for _ in range(2)])
            bt = R.sb([128, 5, 4, 128], F32)
            bfar = R.sb([128, 4, 128], F32)
            bbt = Buf()
            maskT = R.sb([128, NT, 128], BF16)
            bmT = Buf()
            qa_rot = Rot([R.sb([128, 4, 128], BF16) for _ in range(2)])
            P_rot = Rot([R.sb([128, 512], BF16) for _ in range(3)])
            Pm_rot = Rot([R.sb([128, 512], BF16) for _ in range(3)])
            den = R.sb([128, 4], F32)
            bden = Buf()
            L_rot = Rot([PS[0], PS[1], PS[2], PS[3]])
            y_rot = Rot([PS[6], PS[7]])
            for g in range(2):
                op("sync", lambda e, g=g: e.dma_start(out=kT[:], in_=kaT_s[2 * g:2 * g + 2].rearrange("o p t -> p o t")), writes=[bkv], dma=True)
                op("gpsimd", lambda e: e.memset(va_g[:, :, :, 64:65], 1.0), writes=[bkv])
                for n8 in range(0, NT, 8):
                    ne = min(NT, n8 + 8)
                    vt, bvt = vtmp_rot.next()
                    op("sync", lambda e, g=g, n8=n8, ne=ne, vt=vt: e.dma_start(out=vt[:, 0:ne - n8, :], in_=va_s[n8 * 128:ne * 128, g * 256:(g + 1) * 256].rearrange("(n p) c -> p n c", p=128)), writes=[bvt], dma=True)
                    op("gpsimd", lambda e, n8=n8, ne=ne, vt=vt: e.tensor_copy(out=va_g[:, n8:ne, :, 0:64], in_=vt[:, 0:ne - n8, :].rearrange("p n (h d) -> p n h d", h=4)), reads=[bvt], writes=[bkv])
                op("sync", lambda e, g=g: e.dma_start(out=bt[:], in_=bias_d[0:5, :, 4 * g:4 * g + 4, :].rearrange("r p h t -> p r h t")), writes=[bbt], dma=True)
                op("sync", lambda e, g=g: e.dma_start(out=bfar[:], in_=bias_d[5, :, 4 * g:4 * g + 4, :]), writes=[bbt], dma=True)
                for r5 in range(5):
                    op("vector", lambda e, r5=r5: e.tensor_tensor(out=bt[:, r5, :, :], in0=bt[:, r5, :, :], in1=bfar[:], op=ALU.subtract), reads=[bbt], writes=[bbt])
                for m in range(NG):
                    jtop = 4 * m + 3
                    nblk = jtop + 1
                    qa_t, bqa = qa_rot.next()
                    yps, byps = y_rot.next()
                    op("sync", lambda e, qa_t=qa_t, m=m, g=g: e.dma_start(out=qa_t[:], in_=q_s[4 * g:4 * g + 4, :, m * 128:(m + 1) * 128].rearrange("o p t -> p o t")), writes=[bqa], dma=True)
                    for b0 in range(0, nblk, 8):
                        nb = min(8, nblk - b0)
                        op("sync", lambda e, m=m, b0=b0, nb=nb: e.dma_start(out=maskT[:, b0:b0 + nb, :], in_=maskT_s[m, b0:b0 + nb].rearrange("n p t -> p n t")), writes=[bmT], dma=True)
                    cctx = {}

                    def cstage1(jb, m=m, qa_t=qa_t, bqa=bqa):
                        r = jb - 4 * m
                        Lps, bL = L_rot.next()
                        Pt, bP = P_rot.next()
                        cctx[jb] = (Pt, bP)
                        near = r >= -1
                        if near:
                            op("tensor", lambda e, Lps=Lps, r=r: e.matmul(Lps[:], lhsT=ident_f, rhs=bt[:, r + 1, :, :].rearrange("p h t -> p (h t)"), start=True, stop=False, skip_group_check=True), reads=[bbt, bconst], writes=[bL])
                        for h in range(4):
                            op("tensor", lambda e, Lps=Lps, h=h, jb=jb, near=near: e.matmul(Lps[:, h * 128:(h + 1) * 128], lhsT=kT[:, h // 2, jb * 128:(jb + 1) * 128], rhs=qa_t[:, h, :], start=(h == 0 and not near), stop=(h == 3), skip_group_check=True),
                               reads=[bkv, bqa], writes=[bL])
                        op("scalar", lambda e, Lps=Lps, Pt=Pt: e.activation(out=Pt[:], in_=Lps[:], func=AF.Exp), reads=[bL], writes=[bP])

                    def cstage2(jb, yps=yps, byps=byps, nblk=nblk):
                        Pt, bP = cctx.pop(jb)
                        Pm, bPm = Pm_rot.next()
                        op("vector", lambda e, Pt=Pt, Pm=Pm, jb=jb: e.tensor_tensor(out=Pm[:].rearrange("p (h t) -> p h t", h=4), in0=Pt[:].rearrange("p (h t) -> p h t", h=4), in1=maskT[:, jb, :].unsqueeze(1).to_broadcast([128, 4, 128]), op=ALU.mult),
                           reads=[bP, bmT], writes=[bPm])
                        for h in range(4):
                            op("tensor", lambda e, Pm=Pm, h=h, jb=jb: e.matmul(yps[:, h * 65:(h + 1) * 65], lhsT=Pm[:, h * 128:(h + 1) * 128], rhs=va_g[:, jb, h, :], start=(jb == 0 and h == 0), stop=(jb == nblk - 1), skip_group_check=True),
                               reads=[bPm, bkv], writes=[byps])

                    for i in range(nblk + 1):
                        if i < nblk:
                            cstage1(i)
                        if i >= 1:
                            cstage2(i - 1)
                    op("vector", lambda e, yps=yps: e.tensor_copy(out=den[:], in_=yps[:, 0:260].rearrange("p (h c) -> p h c", h=4)[:, :, 64]), reads=[byps], writes=[bden])
                    op("vector", lambda e: e.reciprocal(out=den[:], in_=den[:]), reads=[bden], writes=[bden])
                    for h in range(4):
                        op("vector", lambda e, yps=yps, h=h, m=m, g=g: e.tensor_scalar(out=ya_sb[:, m, g * 256 + h * 64:g * 256 + (h + 1) * 64], in0=yps[:, h * 65:h * 65 + 64], scalar1=den[:, h:h + 1], scalar2=None, op0=ALU.mult),
                           reads=[byps, bden], writes=[bya])
            R.barrier()
            R.release(mkC)
            if dbg:
                yaf = R.sb([128, NG, 512], F32)
                bt2 = Buf()
                op("vector", lambda e: e.tensor_copy(out=yaf[:], in_=ya_sb[:]), reads=[bya], writes=[bt2])
                op("sync", lambda e: e.dma_start(out=dbg_d["ya"], in_=yaf[:]), reads=[bt2], dma=True)
                R.barrier()
                R.release(mkC)

        if stop_after >= "D":
            mkD = R.mark()
            wpa = R.sb([128, 4, 1024], BF16)
            wpb = R.sb([128, 4, 1024], BF16)
            wout = R.sb([128, 8, 1024], BF16)
            bw = Buf()
            for wt, wd in ((wpa, wpa_d), (wpb, wpb_d), (wout, wout_d)):
                op("gpsimd", lambda e, wt=wt, wd=wd: e.dma_start(out=wt[:], in_=wd.rearrange("(c p) n -> p c n", p=128)), writes=[bw], dma=True)
            yT_rot = Rot([R.sb([128, 8, 128], BF16) for _ in range(2)])
            gt_rot = Rot([R.sb([128, 16, 128], BF16) for _ in range(2)])
            t1_rot = Rot([R.sb([128, 512], F32) for _ in range(2)])
            t2_rot = Rot([R.sb([128, 512], F32) for _ in range(2)])
            mg_rot = Rot([R.sb([128, 8, 128], BF16) for _ in range(2)])
            xo_rot = Rot([R.sb([128, 1024], F32) for _ in range(2)])
            x2_rot = Rot([R.sb([128, 1024], F32) for _ in range(2)])
            tpD = Rot([PS[0]])
            paD = Rot([PS[1], PS[2]])
            pbD = Rot([PS[3], PS[4]])
            oD = Rot([PS[5], PS[6]])
            for m in range(NG):
                yT, byT = yT_rot.next()
                gt, bgt = gt_rot.next()
                mg, bmg = mg_rot.next()
                xo, bxo = xo_rot.next()
                x2t, bx2 = x2_rot.next()
                op("sync", lambda e, gt=gt, m=m: e.dma_start(out=gt[:], in_=gT_s[:, :, m * 128:(m + 1) * 128].rearrange("o p t -> p o t")), writes=[bgt], dma=True)
                op("sync", lambda e, xo=xo, m=m: e.dma_start(out=xo[:], in_=xo_d[m * 128:(m + 1) * 128, :]), writes=[bxo], dma=True)
                tp, btp = tpD.next()
                tpb = tp[:].bitcast(BF16)
                for c in range(4):
                    op("tensor", lambda e, tpb=tpb, c=c, m=m: e.transpose(out=tpb[:, c * 128:(c + 1) * 128], in_=ya_sb[:, m, c * 128:(c + 1) * 128], identity=ident_b[:]), reads=[bya, bconst], writes=[btp])
                    op("tensor", lambda e, tpb=tpb, c=c, m=m: e.transpose(out=tpb[:, (4 + c) * 128:(5 + c) * 128], in_=yb_sb[:, m, c * 128:(c + 1) * 128], identity=ident_b[:]), reads=[byb, bconst], writes=[btp])
                op("scalar", lambda e, tpb=tpb, yT=yT: e.copy(out=yT[:].rearrange("p c t -> p (c t)"), in_=tpb[:, 0:1024]), reads=[btp], writes=[byT])
                for half in range(2):
                    pa, bpa = paD.next()
                    pb, bpb = pbD.next()
                    t1, bt1 = t1_rot.next()
                    t2, bt2_ = t2_rot.next()
                    for bq_ in range(4):
                        blk = half * 4 + bq_
                        for c in range(4):
                            op("tensor", lambda e, pa=pa, bq_=bq_, blk=blk, c=c, yT=yT: e.matmul(pa[:, bq_ * 128:(bq_ + 1) * 128], lhsT=wpa[:, c, blk * 128:(blk + 1) * 128], rhs=yT[:, c, :], start=(c == 0), stop=(c == 3)), reads=[bw, byT], writes=[bpa])
                        for c in range(4):
                            op("tensor", lambda e, pb=pb, bq_=bq_, blk=blk, c=c, yT=yT: e.matmul(pb[:, bq_ * 128:(bq_ + 1) * 128], lhsT=wpb[:, c, blk * 128:(blk + 1) * 128], rhs=yT[:, 4 + c, :], start=(c == 0), stop=(c == 3)), reads=[bw, byT], writes=[bpb])
                    op("vector", lambda e, pa=pa, t1=t1, gt=gt, half=half: e.tensor_tensor(out=t1[:], in0=pa[:], in1=gt[:, half * 4:half * 4 + 4, :].rearrange("p o t -> p (o t)"), op=ALU.mult), reads=[bpa, bgt], writes=[bt1])
                    op("vector", lambda e, pb=pb, t2=t2, gt=gt, half=half: e.tensor_tensor(out=t2[:], in0=pb[:], in1=gt[:, 8 + half * 4:12 + half * 4, :].rearrange("p o t -> p (o t)"), op=ALU.mult), reads=[bpb, bgt], writes=[bt2_])
                    op("gpsimd", lambda e, t1=t1, t2=t2, mg=mg, half=half: e.tensor_tensor(out=mg[:, half * 4:half * 4 + 4, :].rearrange("p o t -> p (o t)"), in0=t1[:], in1=t2[:], op=ALU.add), reads=[bt1, bt2_], writes=[bmg])
                for half in range(2):
                    o2, bo2 = oD.next()
                    for c in range(8):
                        op("tensor", lambda e, o2=o2, c=c, mg=mg, half=half: e.matmul(o2[:], lhsT=mg[:, c, :], rhs=wout[:, c, half * 512:(half + 1) * 512], start=(c == 0), stop=(c == 7)), reads=[bw, bmg], writes=[bo2])
                    hs = slice(half * 512, (half + 1) * 512)
                    op("vector", lambda e, o2=o2, x2t=x2t, hs=hs: e.tensor_tensor(out=x2t[:, hs], in0=o2[:], in1=gate1[:, hs], op=ALU.mult), reads=[bo2, bmod], writes=[bx2])
                    op("gpsimd", lambda e, x2t=x2t, xo=xo, hs=hs: e.tensor_tensor(out=x2t[:, hs], in0=x2t[:, hs], in1=xo[:, hs], op=ALU.add), reads=[bx2, bxo], writes=[bx2])
                op("sync", lambda e, x2t=x2t, m=m: e.dma_start(out=x2_s[m * 128:(m + 1) * 128, :], in_=x2t[:]), reads=[bx2], dma=True)
                if dbg:
                    op("sync", lambda e, x2t=x2t, m=m: e.dma_start(out=dbg_d["x2"][m * 128:(m + 1) * 128, :], in_=x2t[:]), reads=[bx2], dma=True)
            R.barrier()
            R.release(mkD)

        if stop_after >= "E":
            R.release(mkY)
            mkE = R.mark()
            TGT = min(2, NG)
            TG = 128 * TGT
            wqb = R.sb([128, 8, 2048], BF16)
            skb = R.sb([128, 16, 128], BF16)
            bwE = Buf()
            op("gpsimd", lambda e: e.dma_start(out=wqb[:], in_=wq_d.rearrange("(c p) n -> p c n", p=128)), writes=[bwE], dma=True)
            op("gpsimd", lambda e: e.dma_start(out=skb[:], in_=skT_d), writes=[bwE], dma=True)
            x2g = R.sb([128, TGT, 1024], F32)
            bx2g = Buf()
            ntmp = R.sb([128, 1024], F32)
            bnt = Buf()
            hn2 = R.sb([128, TGT, 1024], BF16)
            bhn2 = Buf()
            hn2T = R.sb([128, 8, TG], BF16)
            bhn2T = Buf()
            ssE = R.sb([128, TGT], F32)
            bssE = Buf()
            junk = R.sb([128, 1024], BF16)
            bjk = Buf()
            qT_sb = R.sb([128, 16, TG], BF16)
            bqT = Buf()
            s_sb = R.sb([128, TGT, 16, 128], F32)
            bs = Buf()
            E_sb = s_sb
            bE = bs
            E1s = R.sb([128, TGT, 8, 128], F32)
            bE1s = Buf()
            tmpk = R.sb([128, 256], F32)
            btk = Buf()
            m8e = R.sb([128, 16, 8], F32)
            negmx = R.sb([128, 16], F32)
            bm8e = Buf()
            Etop = R.sb([128, 16, 16], F32)
            bEt = Buf()
            cand = R.sb([128, 8, 256], F32)
            bcand = Buf()
            ctop = R.sb([128, 8, 16], F32)
            bct = Buf()
            Zs = R.sb([128, 8], F32)
            bZ = Buf()
            E1topS = R.sb([128, 8, 16], F32)
            bE1t = Buf()
            theta = R.sb([128, TGT, 8], F32)
            bth = Buf()
            Pp_rot = Rot([R.sb([128, 512], F32) for _ in range(3)])
            Wh_rot = Rot([R.sb([128, 512], BF16) for _ in range(3)])
            G_rot = Rot([R.sb([128, 4 * TG], BF16) for _ in range(2)])
            coef_rot = Rot([R.sb([128, 4 * TG], BF16) for _ in range(2)])
            u_rot = Rot([R.sb([128, 8, 512], BF16) for _ in range(2)])
            v_rot = Rot([R.sb([128, 4, 1024], BF16) for _ in range(2)])
            x3 = R.sb([128, 1024], F32)
            bx3 = Buf()
            PSB = [Buf() for _ in range(8)]
            NWB = TGT
            WTb = list(range(0, NWB))
            PRb = list(range(NWB, 2 * NWB))
            Fb = list(range(2 * NWB, 2 * NWB + 2 * TGT))
            misc = Rot([0, 1, 2, 3][:2 * NWB])

            def cand_top(first):
                for h in range(8):
                    e1 = (Etop if first else E1topS)
                    i1 = (2 * h if first else h)
                    op("vector", lambda e, h=h, e1=e1, i1=i1: e.tensor_tensor(out=cand[:, h, :].rearrange("p (a b) -> p a b", a=16), in0=e1[:, i1, :].unsqueeze(2).to_broadcast([128, 16, 16]), in1=Etop[:, 2 * h + 1, :].unsqueeze(1).to_broadcast([128, 16, 16]), op=ALU.mult),
                       reads=[bEt, bE1t], writes=[bcand])
                for h in range(8):
                    op("vector", lambda e, h=h: e.max(out=ctop[:, h, 0:8], in_=cand[:, h, :]), reads=[bcand], writes=[bct])
                    op("vector", lambda e, h=h: e.match_replace(out=tmpk[:, 0:256], in_to_replace=ctop[:, h, 0:8], in_values=cand[:, h, :], imm_value=-1.0), reads=[bcand, bct], writes=[btk])
                    op("vector", lambda e, h=h: e.max(out=ctop[:, h, 8:16], in_=tmpk[:, 0:256]), reads=[btk], writes=[bct])

            for gi in range(NG // TGT):
                op("sync", lambda e, gi=gi: e.dma_start(out=x2g[:], in_=x2_s[gi * TG:(gi + 1) * TG, :].rearrange("(tt p) d -> p tt d", p=128)), writes=[bx2g], dma=True)
                for tt in range(TGT):
                    op("scalar", lambda e, tt=tt: e.activation(out=junk[:], in_=x2g[:, tt, :], func=AF.Square, accum_out=ssE[:, tt:tt + 1]), reads=[bx2g], writes=[bjk, bssE])
                op("vector", lambda e: e.tensor_scalar(out=ssE[:], in0=ssE[:], scalar1=1.0 / 1024, scalar2=1e-6, op0=ALU.mult, op1=ALU.add), reads=[bssE], writes=[bssE])
                op("scalar", lambda e: e.sqrt(out=ssE[:], in_=ssE[:]), reads=[bssE], writes=[bssE])
                op("vector", lambda e: e.reciprocal(out=ssE[:], in_=ssE[:]), reads=[bssE], writes=[bssE])
                for tt in range(TGT):
                    op("vector", lambda e, tt=tt: e.scalar_tensor_tensor(out=ntmp[:], in0=x2g[:, tt, :], scalar=ssE[:, tt:tt + 1], in1=gmod2[:], op0=ALU.mult, op1=ALU.mult), reads=[bx2g, bssE, bgm], writes=[bnt])
                    op("gpsimd", lambda e, tt=tt: e.tensor_tensor(out=hn2[:, tt, :], in0=ntmp[:], in1=shift2, op=ALU.add), reads=[bnt, bmod], writes=[bhn2])
                    bi = misc.next()[0]
                    tpb = PS[bi][:].bitcast(BF16)
                    for c in range(8):
                        op("tensor", lambda e, tpb=tpb, tt=tt, c=c: e.transpose(out=tpb[:, c * 128:(c + 1) * 128], in_=hn2[:, tt, c * 128:(c + 1) * 128], identity=ident_b[:]), reads=[bhn2, bconst], writes=[PSB[bi]])
                    op("scalar", lambda e, tpb=tpb, tt=tt: e.copy(out=hn2T[:, :, tt * 128:(tt + 1) * 128], in_=tpb[:, 0:1024].rearrange("p (c t) -> p c t", c=8)), reads=[PSB[bi]], writes=[bhn2T])
                for hc in range(16):
                    bi = misc.next()[0]
                    for c in range(8):
                        op("tensor", lambda e, bi=bi, hc=hc, c=c: e.matmul(PS[bi][:, 0:TG], lhsT=wqb[:, c, hc * 128:(hc + 1) * 128], rhs=hn2T[:, c, :], start=(c == 0), stop=(c == 7)), reads=[bwE, bhn2T], writes=[PSB[bi]])
                    if hc % 2:
                        op("scalar", lambda e, bi=bi, hc=hc: e.copy(out=qT_sb[:, hc, :], in_=PS[bi][:, 0:TG]), reads=[PSB[bi]], writes=[bqT])
                    else:
                        op("vector", lambda e, bi=bi, hc=hc: e.tensor_copy(out=qT_sb[:, hc, :], in_=PS[bi][:, 0:TG]), reads=[PSB[bi]], writes=[bqT])
                for tt in range(TGT):
                    for hcg in range(4):
                        bi = misc.next()[0]
                        for hq in range(4):
                            hc = hcg * 4 + hq
                            op("tensor", lambda e, bi=bi, hq=hq, hc=hc, tt=tt: e.matmul(PS[bi][:, hq * 128:(hq + 1) * 128], lhsT=qT_sb[:, hc, tt * 128:(tt + 1) * 128], rhs=skb[:, hc, :], start=True, stop=True), reads=[bqT, bwE], writes=[PSB[bi]])
                        op("scalar", lambda e, bi=bi, hcg=hcg, tt=tt: e.copy(out=s_sb[:, tt, hcg * 4:(hcg + 1) * 4, :].rearrange("p h n -> p (h n)"), in_=PS[bi][:]), reads=[PSB[bi]], writes=[bs])
                for tt in range(TGT):
                    for hc in range(16):
                        op("vector", lambda e, hc=hc, tt=tt: e.max(out=m8e[:, hc, :], in_=s_sb[:, tt, hc, :]), reads=[bs], writes=[bm8e])
                    op("vector", lambda e: e.tensor_scalar(out=negmx[:], in0=m8e[:, :, 0], scalar1=-1.0, scalar2=None, op0=ALU.mult), reads=[bm8e], writes=[bm8e])
                    for hc in range(16):
                        op("scalar", lambda e, hc=hc, tt=tt: e.activation(out=E_sb[:, tt, hc, :], in_=s_sb[:, tt, hc, :], func=AF.Exp, bias=negmx[:, hc:hc + 1]), reads=[bm8e], writes=[bE])
                    for hc in range(16):
                        op("vector", lambda e, hc=hc, tt=tt: e.max(out=Etop[:, hc, 0:8], in_=E_sb[:, tt, hc, :]), reads=[bE], writes=[bEt])
                        op("vector", lambda e, hc=hc, tt=tt: e.match_replace(out=tmpk[:, 0:128], in_to_replace=Etop[:, hc, 0:8], in_values=E_sb[:, tt, hc, :], imm_value=-1.0), reads=[bE, bEt], writes=[btk])
                        op("vector", lambda e, hc=hc: e.max(out=Etop[:, hc, 8:16], in_=tmpk[:, 0:128]), reads=[btk], writes=[bEt])
                    cand_top(True)
                    op("vector", lambda e: e.tensor_reduce(out=Zs[:], in_=ctop[:], axis=AX.X, op=ALU.add), reads=[bct], writes=[bZ])
                    op("vector", lambda e: e.reciprocal(out=Zs[:], in_=Zs[:]), reads=[bZ], writes=[bZ])
                    op("vector", lambda e: e.tensor_tensor(out=E1topS[:], in0=Etop[:].rearrange("p (h c) k -> p h c k", c=2)[:, :, 0, :], in1=Zs[:].unsqueeze(2).to_broadcast([128, 8, 16]), op=ALU.mult), reads=[bEt, bZ], writes=[bE1t])
                    op("vector", lambda e, tt=tt: e.tensor_tensor(out=E1s[:, tt, :, :], in0=E_sb[:, tt, :, :].rearrange("p (h c) n -> p h c n", c=2)[:, :, 0, :], in1=Zs[:].unsqueeze(2).to_broadcast([128, 8, 128]), op=ALU.mult), reads=[bE, bZ], writes=[bE1s])
                    cand_top(False)
                    op("vector", lambda e, tt=tt: e.tensor_copy(out=theta[:, tt, :], in_=ctop[:, :, 15]), reads=[bct], writes=[bth])
                NCH = 32
                for ch in range(NCH):
                    e0 = ch * 512
                    ut, but = u_rot.next()
                    vt, bvt = v_rot.next()
                    Gt, bG = G_rot.next()
                    cf, bcf = coef_rot.next()
                    op("gpsimd", lambda e, ut=ut, e0=e0: e.dma_start(out=ut[:], in_=uT_d[:, e0:e0 + 512].rearrange("(c p) e -> p c e", p=128)), writes=[but], dma=True)
                    op("gpsimd", lambda e, vt=vt, e0=e0: e.dma_start(out=vt[:], in_=v_d[e0:e0 + 512, :].rearrange("(b p) d -> p b d", p=128)), writes=[bvt], dma=True)
                    first = {bi: True for bi in WTb}
                    for tt in range(TGT):
                        for h in range(8):
                            Pp, bPp = Pp_rot.next()
                            Wh, bWh = Wh_rot.next()
                            op("vector", lambda e, Pp=Pp, tt=tt, h=h, ch=ch: e.tensor_tensor(out=Pp[:].rearrange("p (a b) -> p a b", a=4), in0=E1s[:, tt, h, ch * 4:(ch + 1) * 4].unsqueeze(2).to_broadcast([128, 4, 128]), in1=E_sb[:, tt, 2 * h + 1, :].unsqueeze(1).to_broadcast([128, 4, 128]), op=ALU.mult),
                               reads=[bE1s, bE], writes=[bPp])
                            op("vector", lambda e, Pp=Pp, Wh=Wh, tt=tt, h=h: e.scalar_tensor_tensor(out=Wh[:], in0=Pp[:], scalar=theta[:, tt, h:h + 1], in1=Pp[:], op0=ALU.is_ge, op1=ALU.mult),
                               reads=[bPp, bth], writes=[bWh])
                            for blk in range(4):
                                reg = blk * TGT + tt
                                bi = WTb[reg // 4]
                                col = (reg % 4) * 128
                                st = first[bi]
                                first[bi] = False
                                op("tensor", lambda e, bi=bi, col=col, Wh=Wh, blk=blk, st=st: e.matmul(PS[bi][:, col:col + 128], lhsT=Wh[:, blk * 128:(blk + 1) * 128], rhs=ident_b[:], start=st, stop=False, skip_group_check=True),
                                   reads=[bWh, bconst], writes=[PSB[bi]])
                    for blk in range(4):
                        bi = PRb[(blk * TGT) // 4]
                        col = ((blk * TGT) % 4) * 128
                        for c in range(8):
                            op("tensor", lambda e, bi=bi, col=col, ut=ut, blk=blk, c=c: e.matmul(PS[bi][:, col:col + TG], lhsT=ut[:, c, blk * 128:(blk + 1) * 128], rhs=hn2T[:, c, :], start=(c == 0), stop=(c == 7)),
                               reads=[but, bhn2T], writes=[PSB[bi]])
                    for k, bi in enumerate(PRb):
                        op("scalar", lambda e, bi=bi, k=k, Gt=Gt: e.activation(out=Gt[:, k * 512:(k + 1) * 512], in_=PS[bi][:], func=AF.Gelu), reads=[PSB[bi]], writes=[bG])
                    for k, bi in enumerate(WTb):
                        op("vector", lambda e, bi=bi, k=k, Gt=Gt, cf=cf: e.tensor_tensor(out=cf[:, k * 512:(k + 1) * 512], in0=PS[bi][:], in1=Gt[:, k * 512:(k + 1) * 512], op=ALU.mult), reads=[PSB[bi], bG], writes=[bcf])
                    for blk in range(4):
                        for tt in range(TGT):
                            cc = (blk * TGT + tt) * 128
                            for half in range(2):
                                bi = Fb[tt * 2 + half]
                                op("tensor", lambda e, bi=bi, cf=cf, cc=cc, vt=vt, blk=blk, half=half, ch=ch: e.matmul(PS[bi][:], lhsT=cf[:, cc:cc + 128], rhs=vt[:, blk, half * 512:(half + 1) * 512], start=(ch == 0 and blk == 0), stop=(ch == NCH - 1 and blk == 3)),
                                   reads=[bcf, bvt], writes=[PSB[bi]])
                for tt in range(TGT):
                    for half in range(2):
                        bi = Fb[tt * 2 + half]
                        hs = slice(half * 512, (half + 1) * 512)
                        op("vector", lambda e, bi=bi, hs=hs: e.tensor_tensor(out=x3[:, hs], in0=PS[bi][:], in1=gate2[:, hs], op=ALU.mult), reads=[PSB[bi], bmod], writes=[bx3])
                    op("gpsimd", lambda e, tt=tt: e.tensor_tensor(out=x3[:], in0=x3[:], in1=x2g[:, tt, :], op=ALU.add), reads=[bx3, bx2g], writes=[bx3])
                    op("scalar", lambda e, tt=tt: e.activation(out=junk[:], in_=x3[:], func=AF.Square, accum_out=ssE[:, tt:tt + 1]), reads=[bx3], writes=[bjk, bssE])
                    op("vector", lambda e, tt=tt: e.tensor_scalar(out=ssE[:, tt:tt + 1], in0=ssE[:, tt:tt + 1], scalar1=1.0 / 1024, scalar2=1e-6, op0=ALU.mult, op1=ALU.add), reads=[bssE], writes=[bssE])
                    op("scalar", lambda e, tt=tt: e.sqrt(out=ssE[:, tt:tt + 1], in_=ssE[:, tt:tt + 1]), reads=[bssE], writes=[bssE])
                    op("vector", lambda e, tt=tt: e.reciprocal(out=ssE[:, tt:tt + 1], in_=ssE[:, tt:tt + 1]), reads=[bssE], writes=[bssE])
                    op("vector", lambda e, tt=tt: e.scalar_tensor_tensor(out=ntmp[:], in0=x3[:], scalar=ssE[:, tt:tt + 1], in1=gf_bc[:], op0=ALU.mult, op1=ALU.mult), reads=[bx3, bssE, bgm], writes=[bnt])
                    op("sync", lambda e, gi=gi, tt=tt: e.dma_start(out=out_d[(gi * TGT + tt) * 128:(gi * TGT + tt + 1) * 128, :], in_=ntmp[:]), reads=[bnt], dma=True)
            R.barrier()
            R.release(mkE)

        R.barrier()
        R.emit()
    return nc


def _t5_bucket(n):
    n = np.asarray(n)
    nf = np.maximum(n, 1).astype(np.float32)
    large = 16 + (np.log(nf / np.float32(16)) / np.float32(math.log(128 / 16)) * np.float32(16)).astype(np.int32)
    large = np.minimum(large, 31)
    return np.where(n < 16, n, large)


def make_inputs(inp, NG=16):
    x = np.asarray(inp["x"], np.float32)
    c = np.asarray(inp["c"], np.float32)
    w_in = np.asarray(inp["w_in"], np.float32)[0]
    offs = np.cumsum([0, 512, 512, 512, 512, 64, 8, 512, 512, 512, 1024, 1024])
    seg = {n: w_in[:, offs[i]:offs[i + 1]] for i, n in enumerate(["qa", "ka", "va", "qi", "ki", "wi", "qb", "kb", "vb", "ga", "gb"])}
    wK = np.ascontiguousarray(np.concatenate([seg["ka"], seg["kb"], seg["ki"]], 1))
    wV = np.ascontiguousarray(np.concatenate([seg["va"], seg["vb"]], 1))
    wQ = np.ascontiguousarray(np.concatenate([seg["qa"], seg["qb"], seg["qi"]], 1))
    wG = np.ascontiguousarray(np.concatenate([seg["ga"], seg["gb"]], 1))
    wWi = np.ascontiguousarray(seg["wi"])
    rel_bias = np.asarray(inp["rel_bias"], np.float32)
    sl = np.arange(128)[:, None]
    tl = np.arange(128)[None, :]
    tiles = []
    for k in range(2):
        n = np.maximum(tl + 128 * k - sl, 0)
        tiles.append(rel_bias[_t5_bucket(n)].transpose(0, 2, 1))
    far = rel_bias[np.full((128, 128), 31)].transpose(0, 2, 1)
    a = np.arange(128)
    ones = np.ones((128, 128), np.float32)
    zeros = np.zeros((128, 128), np.float32)
    percore = []
    for j in range(4):
        consts = np.zeros((15, 128, 128), np.float32)
        consts[0] = np.eye(128)
        consts[1] = -1.0 * (a[:, None] >= a[None, :])
        consts[2] = 1.0
        for r in range(4):
            consts[3 + r] = ones if r < j else ((a[:, None] < a[None, :]) if r == j else zeros)
            consts[7 + r] = ones if r < j else ((a[None, :] <= a[:, None]) if r == j else zeros)
            consts[11 + r] = zeros if r < j else (np.where(a[None, :] <= a[:, None], 0.0, NEG) if r == j else np.full((128, 128), NEG))
        bt = []
        for r in range(-1, 4):
            k = j - r
            bt.append(tiles[k] if k in (0, 1) else far)
        bt.append(far)
        percore.append((consts, np.ascontiguousarray(np.stack(bt, 0))))
    sk = np.asarray(inp["peer_sub_keys"], np.float32)[0]
    skT = np.ascontiguousarray(sk.reshape(16, 128, 128).transpose(2, 0, 1))
    uT = np.ascontiguousarray(np.asarray(inp["peer_u"], np.float32)[0].T)
    shared = {
        "w_ada": np.asarray(inp["w_ada"], np.float32)[0], "b_ada": np.asarray(inp["b_ada"], np.float32)[0][None, :],
        "g1": np.asarray(inp["norm1_g"], np.float32)[0][None, :], "g2": np.asarray(inp["norm2_g"], np.float32)[0][None, :],
        "gf": np.asarray(inp["final_g"], np.float32)[None, :],
        "wK": wK, "wV": wV, "wQ": wQ, "wG": wG, "wWi": wWi,
        "wpa": np.asarray(inp["w_proj_a"], np.float32)[0], "wpb": np.asarray(inp["w_proj_b"], np.float32)[0],
        "wout": np.asarray(inp["w_out"], np.float32)[0], "wq": np.asarray(inp["peer_wq"], np.float32)[0],
        "skT": skT, "uT": uT, "pv": np.asarray(inp["peer_v"], np.float32)[0],
    }
    maps = []
    for core in range(8):
        b, j = core // 4, core % 4
        xo = np.ascontiguousarray(x[b][:512 * NG].reshape(NG, 4, 128, 1024)[:, j].reshape(128 * NG, 1024))
        d = dict(shared)
        d["x"] = np.ascontiguousarray(x[b][:512 * NG])
        d["xo"] = xo
        d["cT"] = np.ascontiguousarray(c[b].reshape(8, 128).T)
        d["consts"], d["biasT"] = percore[j]
        maps.append(d)
    return maps


_NC = {}


def kernel(**inputs):
    maps = make_inputs(inputs)
    if "nc" not in _NC:
        _NC["nc"] = build()
    res = run_bass_kernel_spmd(_NC["nc"], maps, core_ids=list(range(8)))
    out = np.zeros((2, 8192, 1024), np.float32)
    ov = out.reshape(2, 16, 4, 128, 1024)
    for core in range(8):
        b, j = core // 4, core % 4
        ov[b, :, j] = np.asarray(res.results[core]["out"], np.float32).reshape(16, 128, 1024)
    return out
```
